# Optimizing a Trainium2 kernel written in Bass

```python
import math
import jax, jax.numpy as jnp
from jax import lax
import numpy as np

D_MODEL = 4096
BATCH = 4
SEQ = 2048
DEPTH = 1
DEC_BATCH = 128
DEC_SEQ = 4
PAST_LEN = 16384
PAGE_SIZE = 128

RET_HEADS = 8
RET_DK = 256
RET_DV = 256
RET_CHUNK = 128
ROPE_BASE = 10000.0
HG_HEADS = 16
HG_DK = 128
HG_DV = 128
HG_CHUNK = 16
D_FF = 11008
CONV_W = 3
EPS = 1e-6

RET_QK = RET_HEADS * RET_DK
RET_V = RET_HEADS * RET_DV
HG_K = HG_HEADS * HG_DK
HG_V = HG_HEADS * HG_DV
IN_COLS = 2 * RET_QK + 2 * RET_V + 2 * HG_K + 2 * HG_V + 2 * D_MODEL

kernel_name = "retnet_hgrn2_gated_parallel_convffn_step"


def rmsnorm(x, g):
    xf = x.astype(jnp.float32)
    y = xf * lax.rsqrt(jnp.mean(xf * xf, axis=-1, keepdims=True) + EPS)
    return (y * g.astype(jnp.float32)).astype(x.dtype)


def split_in(proj):
    widths = [RET_QK, RET_QK, RET_V, RET_V, HG_K, HG_K, HG_V, HG_V, D_MODEL, D_MODEL]
    offsets = np.cumsum(widths)[:-1].tolist()
    return jnp.split(proj, offsets, axis=-1)


def rotary(x, pos):
    half = x.shape[-1] // 2
    inv = ROPE_BASE ** (-jnp.arange(half, dtype=jnp.float32) / half)
    ang = pos[:, None] * inv[None, :]
    cos = jnp.cos(ang)[None, :, None, :]
    sin = jnp.sin(ang)[None, :, None, :]
    x1, x2 = x[..., :half], x[..., half:]
    return jnp.concatenate([x1 * cos - x2 * sin, x1 * sin + x2 * cos], axis=-1)


def to_chunks(a, L):
    B, T, H, d = a.shape
    return a.reshape(B, T // L, L, H, d).transpose(1, 0, 3, 2, 4)


def from_chunks(o):
    n, B, H, L, d = o.shape
    return o.transpose(1, 0, 3, 2, 4).reshape(B, n * L, H, d)


def retention_chunked(q, k, v, s0):
    T, H = q.shape[1], q.shape[2]
    L = math.gcd(T, RET_CHUNK)
    log_g = jnp.log1p(-jnp.exp2(-5.0 - jnp.arange(H, dtype=jnp.float32)))
    idx = jnp.arange(L, dtype=jnp.float32)
    diff = idx[:, None] - idx[None, :]
    causal = diff >= 0
    dmat = jnp.where(causal[None], jnp.exp(jnp.where(causal, diff, 0.0)[None] * log_g[:, None, None]), 0.0)
    q_dec = jnp.exp((idx + 1.0)[None, :] * log_g[:, None])[None, :, :, None]
    k_dec = jnp.exp((L - 1.0 - idx)[None, :] * log_g[:, None])[None, :, :, None]
    s_dec = jnp.exp(L * log_g)[None, :, None, None]
    f32 = jnp.float32
    qc, kc, vc = (to_chunks(a.astype(f32), L) for a in (q, k, v))

    def step(s, xs):
        qi, ki, vi = xs
        scores = jnp.einsum('bhid,bhjd->bhij', qi, ki) * dmat[None]
        o = jnp.einsum('bhij,bhjv->bhiv', scores, vi) + jnp.einsum('bhid,bhdv->bhiv', qi, s) * q_dec
        s = s * s_dec + jnp.einsum('bhjd,bhjv->bhdv', ki * k_dec, vi)
        return s, o

    s, o = lax.scan(step, s0.astype(f32), (qc, kc, vc))
    return from_chunks(o), s


def hgrn2_chunked(q, log_f, k, v, s0):
    T = q.shape[1]
    L = math.gcd(T, HG_CHUNK)
    mask = jnp.tril(jnp.ones((L, L), dtype=bool))[:, :, None]
    f32 = jnp.float32
    qc, lfc, kc, vc = (to_chunks(a.astype(f32), L) for a in (q, log_f, k, v))

    def step(s, xs):
        qi, lfi, ki, vi = xs
        b = jnp.cumsum(lfi, axis=2)
        rel = b[:, :, :, None, :] - b[:, :, None, :, :]
        decay = jnp.exp(jnp.where(mask, rel, -jnp.inf))
        scores = jnp.einsum('bhtk,bhtsk,bhsk->bhts', qi, decay, ki)
        o = jnp.einsum('bhts,bhsv->bhtv', scores, vi) + jnp.einsum('bhtk,bhkv->bhtv', qi * jnp.exp(b), s)
        b_last = b[:, :, -1]
        s = jnp.exp(b_last)[..., None] * s + jnp.einsum('bhsk,bhsv->bhkv', ki * jnp.exp(b_last[:, :, None] - b), vi)
        return s, o

    s, o = lax.scan(step, s0.astype(f32), (qc, lfc, kc, vc))
    return from_chunks(o), s


def head_rmsnorm(o, g, dtype):
    y = o * lax.rsqrt(jnp.mean(o * o, axis=-1, keepdims=True) + EPS)
    return (y * g.astype(jnp.float32)).astype(dtype)


def block(x, pos, s_ret, s_hg, conv_buf, g_mix, w_in, ret_g, hg_g, lb, w_br_ret, w_br_hg, w_out,
          g_ffn, w_gate, conv_w, conv_b, w_up, w_down):
    B, T, _ = x.shape
    dt = x.dtype
    xn = rmsnorm(x, g_mix)
    proj = xn @ w_in
    q_r, k_r, v_r, g_r, f_h, q_h, i_h, g_h, gate_ret, gate_hg = split_in(proj)
    qr = rotary(q_r.reshape(B, T, RET_HEADS, RET_DK).astype(jnp.float32), pos)
    kr = rotary(k_r.reshape(B, T, RET_HEADS, RET_DK).astype(jnp.float32), pos) * (RET_DK ** -0.5)
    vr = v_r.reshape(B, T, RET_HEADS, RET_DV)
    o_r, s_ret_new = retention_chunked(qr, kr, vr, s_ret)
    o_r = head_rmsnorm(o_r, ret_g.reshape(RET_HEADS, RET_DV), dt).reshape(B, T, RET_V) * jax.nn.silu(g_r)
    p_ret = o_r @ w_br_ret
    f = lb + (1.0 - lb) * jax.nn.sigmoid(f_h.astype(jnp.float32))
    log_f = jnp.log(f).reshape(B, T, HG_HEADS, HG_DK)
    kh = (1.0 - f).reshape(B, T, HG_HEADS, HG_DK)
    qh = q_h.reshape(B, T, HG_HEADS, HG_DK)
    vh = jax.nn.silu(i_h).reshape(B, T, HG_HEADS, HG_DV)
    o_h, s_hg_new = hgrn2_chunked(qh, log_f, kh, vh, s_hg)
    o_h = head_rmsnorm(o_h, hg_g.reshape(HG_HEADS, HG_DV), dt).reshape(B, T, HG_V) * jax.nn.silu(g_h)
    p_hg = o_h @ w_br_hg
    merged = jax.nn.sigmoid(gate_ret) * p_ret + jax.nn.sigmoid(gate_hg) * p_hg
    x = x + merged @ w_out
    xn = rmsnorm(x, g_ffn)
    u = xn @ w_gate
    full = jnp.concatenate([conv_buf.astype(u.dtype), u], axis=1)
    c = conv_b + sum(full[:, j:j + T] * conv_w[j] for j in range(CONV_W))
    h = jax.nn.silu(c) * (xn @ w_up)
    x = x + h @ w_down
    conv_new = full[:, T:]
    return x, s_ret_new.astype(dt), s_hg_new.astype(dt), conv_new


def setup_inputs(seed: int = 0) -> dict:
    key = jax.random.key(seed)
    ks = jax.random.split(key, 24)
    f32 = jnp.float32
    nrm = lambda k, shape, s: jax.random.normal(k, shape, f32) * s
    return {
        "x_prompt": nrm(ks[0], (BATCH, SEQ, D_MODEL), 1.0),
        "x_sample": nrm(ks[1], (DEC_BATCH, DEC_SEQ, D_MODEL), 1.0),
        "state_ret": nrm(ks[2], (DEPTH, DEC_BATCH, RET_HEADS, RET_DK, RET_DV), 0.5),
        "state_hgrn": nrm(ks[3], (DEPTH, DEC_BATCH, HG_HEADS, HG_DK, HG_DV), 0.5),
        "state_ffn_conv": nrm(ks[4], (DEPTH, DEC_BATCH, CONV_W - 1, D_FF), 1.0),
        "norm_mix_g": 1.0 + nrm(ks[5], (DEPTH, D_MODEL), 0.02),
        "w_in": nrm(ks[6], (DEPTH, D_MODEL, IN_COLS), D_MODEL ** -0.5),
        "ret_norm_g": 1.0 + nrm(ks[7], (DEPTH, RET_V), 0.02),
        "hg_norm_g": 1.0 + nrm(ks[8], (DEPTH, HG_V), 0.02),
        "hg_lb_logits": nrm(ks[9], (DEPTH + 1, HG_K), 0.5),
        "w_br_ret": nrm(ks[10], (DEPTH, RET_V, D_MODEL), RET_V ** -0.5),
        "w_br_hg": nrm(ks[11], (DEPTH, HG_V, D_MODEL), HG_V ** -0.5),
        "w_out": nrm(ks[12], (DEPTH, D_MODEL, D_MODEL), D_MODEL ** -0.5),
        "norm_ffn_g": 1.0 + nrm(ks[13], (DEPTH, D_MODEL), 0.02),
        "w_gate": nrm(ks[14], (DEPTH, D_MODEL, D_FF), D_MODEL ** -0.5),
        "conv_w": nrm(ks[15], (DEPTH, CONV_W, D_FF), CONV_W ** -0.5),
        "conv_b": nrm(ks[16], (DEPTH, D_FF), 0.02),
        "w_up": nrm(ks[17], (DEPTH, D_MODEL, D_FF), D_MODEL ** -0.5),
        "w_down": nrm(ks[18], (DEPTH, D_FF, D_MODEL), D_FF ** -0.5),
        "final_norm_g": 1.0 + nrm(ks[19], (D_MODEL,), 0.02),
    }


def reference(x_prompt, x_sample, state_ret, state_hgrn, state_ffn_conv, norm_mix_g, w_in, ret_norm_g,
              hg_norm_g, hg_lb_logits, w_br_ret, w_br_hg, w_out, norm_ffn_g, w_gate, conv_w, conv_b,
              w_up, w_down, final_norm_g):
    f32 = jnp.float32
    Bp, Tp, _ = x_prompt.shape
    Ts = x_sample.shape[1]
    pos_p = jnp.arange(Tp, dtype=f32)
    pos_s = PAST_LEN + jnp.arange(Ts, dtype=f32)
    lb_all = jnp.cumsum(jax.nn.softmax(hg_lb_logits.astype(f32), axis=0), axis=0)
    yp, ys = x_prompt, x_sample
    rp, hp, cp, rs, hs, cs = [], [], [], [], [], []
    for l in range(DEPTH):
        params = (norm_mix_g[l], w_in[l], ret_norm_g[l], hg_norm_g[l], lb_all[l], w_br_ret[l], w_br_hg[l],
                  w_out[l], norm_ffn_g[l], w_gate[l], conv_w[l], conv_b[l], w_up[l], w_down[l])
        zr = jnp.zeros((Bp, RET_HEADS, RET_DK, RET_DV), f32)
        zh = jnp.zeros((Bp, HG_HEADS, HG_DK, HG_DV), f32)
        zc = jnp.zeros((Bp, CONV_W - 1, D_FF), x_prompt.dtype)
        yp, r1, h1, c1 = block(yp, pos_p, zr, zh, zc, *params)
        ys, r2, h2, c2 = block(ys, pos_s, state_ret[l], state_hgrn[l], state_ffn_conv[l], *params)
        rp.append(r1); hp.append(h1); cp.append(c1)
        rs.append(r2); hs.append(h2); cs.append(c2)
    y_prompt = rmsnorm(yp, final_norm_g)
    y_sample = rmsnorm(ys, final_norm_g)
    return (y_prompt, y_sample, jnp.stack(rp), jnp.stack(hp), jnp.stack(cp),
            jnp.stack(rs), jnp.stack(hs), jnp.stack(cs))
```

```python
import numpy as np
import concourse.bass as bass
import concourse.mybir as mybir
from concourse.bass_utils import run_bass_kernel_spmd

F32 = mybir.dt.float32
BF16 = mybir.dt.bfloat16
ALU = mybir.AluOpType
AF = mybir.ActivationFunctionType

D = 4096
FF = 11008
NT_ALL = 17
EPS = 1e-6
PAST = 16384
C_QR, C_KR, C_VR, C_GR, C_FH, C_QH, C_IH, C_GH, C_GATE_R, C_GATE_H = (
    0, 2048, 4096, 6144, 8192, 10240, 12288, 14336, 16384, 20480)
GAM = [1.0 - 2.0 ** (-5.0 - h) for h in range(8)]


class Op:
    __slots__ = ("eng", "fn", "deps", "sig", "sigidx", "is_dma", "sem", "semval", "slot", "redirect")

    def __init__(self, eng, fn):
        self.eng = eng
        self.fn = fn
        self.deps = []
        self.sig = False
        self.sigidx = 0
        self.is_dma = False
        self.sem = None
        self.semval = 0
        self.slot = None
        self.redirect = None


class Prog:
    ENGS = ("pe", "act", "dve", "pool", "sp")

    def __init__(self):
        self.streams = {e: [] for e in self.ENGS}
        self.lastw = {}
        self.readers = {}
        self.slot_cnt = {}
        self.slot_last = {}
        self.rr = {}

    def _add(self, op, reads, writes):
        deps = {}
        for t in reads:
            w = self.lastw.get(t)
            if w is not None:
                deps[id(w)] = w
            if t.startswith("ps"):
                for r in self.readers.get(t, {}).values():
                    if r.eng != op.eng:
                        deps[id(r)] = r
        for t in writes:
            w = self.lastw.get(t)
            if w is not None:
                deps[id(w)] = w
            for r in self.readers.get(t, {}).values():
                deps[id(r)] = r
        for t in reads:
            rd = self.readers.setdefault(t, {})
            key = op.eng if not op.is_dma else ("dma", op.slot)
            rd[key] = op
        for t in writes:
            self.lastw[t] = op
            self.readers[t] = {}
        deps.pop(id(op), None)
        op.deps = list(deps.values())
        self.streams[op.eng].append(op)
        return op

    def op(self, eng, fn, reads=(), writes=()):
        return self._add(Op(eng, fn), reads, writes)

    SLOT_RR = {"c": 4, "x": 2, "st": 3, "so": 3, "og": 2, "y": 2, "cv": 2, "m": 1, "w0": 1, "w1": 1, "w2": 1}

    def dma(self, eng, slot, fn, reads=(), writes=()):
        n = self.SLOT_RR[slot]
        k = self.rr.get(slot, 0)
        self.rr[slot] = k + 1
        slot = "%s_%d" % (slot, k % n)
        o = Op(eng, fn)
        o.is_dma = True
        o.slot = slot
        self.slot_cnt[slot] = self.slot_cnt.get(slot, 0) + 16
        o.semval = self.slot_cnt[slot]
        prev = self.slot_last.get(slot)
        self.slot_last[slot] = o
        self._add(o, reads, writes)
        if prev is not None and all(d is not prev for d in o.deps):
            o.deps.append(prev)
        return o

    @classmethod
    def all_slots(cls):
        return ["%s_%d" % (s, i) for s, n in cls.SLOT_RR.items() for i in range(n)]

    def barrier(self):
        lasts = []
        for e in self.ENGS:
            for o in reversed(self.streams[e]):
                if not o.is_dma and o.fn is not None:
                    lasts.append(o)
                    break
        dmas = list(self.slot_last.values())
        for e in self.ENGS:
            b = Op(e, None)
            for l in lasts:
                if l.eng != e:
                    b.deps.append(l)
            b.deps.extend(dmas)
            self.streams[e].append(b)
        self.lastw = {}
        self.readers = {}

    def emit(self, nc, block, esem, dsem):
        for e in self.ENGS:
            for o in self.streams[e]:
                nd = []
                for d in o.deps:
                    if not d.is_dma and d.redirect is not None:
                        d = d.redirect
                    if d is o:
                        continue
                    nd.append(d)
                    if d.is_dma:
                        continue
                    if d.eng == o.eng and o.eng == "pe" and not o.is_dma:
                        continue
                    d.sig = True
                o.deps = nd
        for e in self.ENGS:
            c = 0
            for o in self.streams[e]:
                if o.sig and not o.is_dma and o.fn is not None:
                    c += 1
                    o.sigidx = c
        handles = {"pe": "tensor", "act": "scalar", "dve": "vector", "pool": "gpsimd", "sp": "sync"}

        def run(e, eng):
            waited = {}
            for o in self.streams[e]:
                for d in o.deps:
                    if d.is_dma:
                        key, val, sem = ("d", d.slot), d.semval, dsem[d.slot]
                    else:
                        if d.eng == e and e == "pe":
                            continue
                        key, val, sem = ("e", d.eng), d.sigidx, esem[d.eng]
                    if waited.get(key, 0) >= val:
                        continue
                    waited[key] = val
                    eng.wait_ge(sem, val)
                if o.fn is None:
                    continue
                ins = o.fn(eng)
                if o.is_dma:
                    ins.then_inc(dsem[o.slot], 16)
                elif o.sig:
                    ins.then_inc(esem[e], 1)

        @block.tensor
        def _(eng):
            run("pe", eng)

        @block.scalar
        def _(eng):
            run("act", eng)

        @block.vector
        def _(eng):
            run("dve", eng)

        @block.gpsimd
        def _(eng):
            run("pool", eng)

        @block.sync
        def _(eng):
            run("sp", eng)


def _consts():
    c = {}
    idx = np.arange(128)
    diff = idx[None, :] - idx[:, None]
    dm = np.zeros((128, 8, 128), np.float32)
    dm4 = np.zeros((128, 8, 128), np.float32)
    qd = np.zeros((128, 8), np.float32)
    kd = np.zeros((128, 8), np.float32)
    qd4 = np.zeros((128, 8), np.float32)
    kd4 = np.zeros((128, 8), np.float32)
    same4 = (idx[None, :] // 4) == (idx[:, None] // 4)
    for h in range(8):
        lg = np.log1p(-np.exp2(np.float32(-5.0 - h))).astype(np.float32)
        dm[:, h, :] = np.where(diff >= 0, np.exp(np.where(diff >= 0, diff, 0).astype(np.float32) * lg), 0.0)
        dm4[:, h, :] = np.where((diff >= 0) & same4, np.exp(np.where(diff >= 0, diff, 0).astype(np.float32) * lg), 0.0)
        qd[:, h] = np.exp((idx + 1.0).astype(np.float32) * lg)
        kd[:, h] = np.exp((127.0 - idx).astype(np.float32) * lg)
        qd4[:, h] = np.exp(((idx % 4) + 1.0).astype(np.float32) * lg)
        kd4[:, h] = np.exp((3.0 - (idx % 4)).astype(np.float32) * lg)
    c["dm"] = dm
    c["dm4"] = dm4
    c["dec"] = np.concatenate([qd, kd, qd4, kd4], axis=1).astype(np.float32)
    same16 = (idx[None, :] // 16) == (idx[:, None] // 16)
    c["tri16"] = ((diff >= 0) & same16).astype(np.float32)
    c["blk16"] = same16.astype(np.float32)
    c["tri4"] = ((diff >= 0) & same4).astype(np.float32)
    c["blk4"] = same4.astype(np.float32)
    cm16 = np.zeros((8, 128), np.float32)
    for ch in range(8):
        cm16[ch, ch * 16:(ch + 1) * 16] = 1.0
    cm4 = np.zeros((16, 128), np.float32)
    for s in range(16):
        cm4[s, s * 4:(s + 1) * 4] = 1.0
    c["cm16"] = np.broadcast_to(cm16.reshape(1, 8 * 128), (128, 1024)).copy()
    c["cm4"] = np.broadcast_to(cm4.reshape(1, 16 * 128), (128, 2048)).copy()
    c["cmT"] = np.concatenate([cm16.T, cm4.T], axis=1).astype(np.float32).copy()
    cmT64 = np.zeros((128, 64), np.float32)
    cmT64[:, 0:8] = cm16.T
    cmT64[:, 32:48] = cm4.T
    c["cmT64"] = cmT64
    c["ident"] = np.eye(128, dtype=np.float32)
    return c


def _rope_tables(pos):
    half = 128
    inv = (np.float32(10000.0) ** (-np.arange(half, dtype=np.float32) / np.float32(half))).astype(np.float32)
    ang = (pos.astype(np.float32)[:, None] * inv[None, :]).astype(np.float32)
    return np.cos(ang).astype(np.float32), np.sin(ang).astype(np.float32)


def build_program():
    nc = bass.Bass("TRN2", target_bir_lowering=False)

    def din(name, shape, dt=F32):
        return nc.dram_tensor(name, list(shape), dt, kind="ExternalInput").ap()

    def dout(name, shape, dt=F32):
        return nc.dram_tensor(name, list(shape), dt, kind="ExternalOutput").ap()

    xa = din("xa", [NT_ALL * 128, D])
    w_in = din("w_in", [D, 24576])
    w_br_ret = din("w_br_ret", [2048, D])
    w_br_hg = din("w_br_hg", [2048, D])
    w_out = din("w_out", [D, D])
    w_gate = din("w_gate", [D, FF])
    w_up = din("w_up", [D, FF])
    w_down = din("w_down", [FF, D])
    g_mix = din("g_mix", [128, 32])
    g_ffn = din("g_ffn", [128, 32])
    cwb_d = din("cwb", [128, 86 * 4])
    g_fin = din("g_fin", [1, D])
    ret_g = din("ret_g", [1, 2048])
    hg_g = din("hg_g", [1, 2048])
    lbl = din("lbl", [2, 2048])
    sret = din("sret", [16, 8, 256, 256])
    shg = din("shg", [16, 16, 128, 128])
    sconv = din("sconv", [32, FF])
    cosT = din("cosT", [NT_ALL, 128, 128])
    sinT = din("sinT", [NT_ALL, 128, 128])
    k_dm = din("k_dm", [128, 8, 128])
    k_dm4 = din("k_dm4", [128, 8, 128])
    k_dec = din("k_dec", [128, 32])
    k_tri16 = din("k_tri16", [128, 128])
    k_blk16 = din("k_blk16", [128, 128])
    k_tri4 = din("k_tri4", [128, 128])
    k_blk4 = din("k_blk4", [128, 128])
    k_cm16 = din("k_cm16", [128, 1024])
    k_cm4 = din("k_cm4", [128, 2048])
    k_cmT = din("k_cmT", [128, 24])
    k_cmT64 = din("k_cmT64", [128, 64])
    k_ident = din("k_ident", [128, 128])

    y_o = dout("y_o", [1088, D])
    retp_o = dout("retp_o", [8, 256, 256])
    hgp_o = dout("hgp_o", [16, 128, 128])
    convp_o = dout("convp_o", [2, FF])
    rets_o = dout("rets_o", [16, 8, 256, 256])
    hgs_o = dout("hgs_o", [16, 16, 128, 128])
    convs_o = dout("convs_o", [32, FF])

    og_scr = nc.dram_tensor("og_scr", [32, 128, 1280], BF16, kind="Internal").ap()
    sr_scr = nc.dram_tensor("sr_scr", [8, 256, 256], F32, kind="Internal").ap()
    sh_scr = nc.dram_tensor("sh_scr", [16, 128, 128], F32, kind="Internal").ap()

    P = Prog()
    from contextlib import ExitStack
    es = ExitStack()

    def sb(name, shape, dt=F32):
        return es.enter_context(nc.sbuf_tensor(name, list(shape), dt))

    AW = 46 * 1024
    arena = sb("arena", [128, AW])
    ident_f = sb("ident_f", [128, 128])
    ident_b = sb("ident_b", [128, 128], BF16)
    dm = sb("dm", [128, 8, 128])
    dm4 = sb("dm4", [128, 8, 128])
    dec = sb("dec", [128, 32])
    tri16 = sb("tri16", [128, 128])
    blk16 = sb("blk16", [128, 128])
    tri4 = sb("tri4", [128, 128])
    blk4 = sb("blk4", [128, 128])
    cm16 = sb("cm16", [128, 8, 128], BF16)
    cm4 = sb("cm4", [128, 16, 128], BF16)
    cmT = sb("cmT", [128, 24])
    tri16b = sb("tri16b", [128, 128], BF16)
    blk16b = sb("blk16b", [128, 128], BF16)
    tri4b = sb("tri4b", [128, 128], BF16)
    blk4b = sb("blk4b", [128, 128], BF16)
    cmTb = sb("cmTb", [128, 64], BF16)
    gmixT = sb("gmixT", [128, 32])
    gffnT = sb("gffnT", [128, 32])
    small = sb("small", [128, 64])
    carry = sb("carry", [128, 86, 2])
    cwb = sb("cwb_s", [128, 86, 4])

    psum = [es.enter_context(nc.psum_tensor("ps%d" % i, [128, 512], F32)) for i in range(6)]
    pskv2 = [es.enter_context(nc.psum_tensor("pskv%d" % i, [128, 512], F32)) for i in range(2)]

    def kvview(c):
        return pskv2[c // 4][:, (c % 4) * 128:(c % 4 + 1) * 128]

    def a32(off, shape):
        n = int(np.prod(shape))
        v = arena[:, off:off + n]
        if len(shape) == 2:
            return v.rearrange("p (a b) -> p a b", a=shape[0])
        if len(shape) == 3:
            return v.rearrange("p (a b c) -> p a b c", a=shape[0], b=shape[1])
        return v

    def a16(off, shape):
        n = int(np.prod(shape))
        assert n % 2 == 0
        v = arena[:, off:off + n // 2].bitcast(BF16)
        if len(shape) == 2:
            return v.rearrange("p (a b) -> p a b", a=shape[0])
        if len(shape) == 3:
            return v.rearrange("p (a b c) -> p a b c", a=shape[0], b=shape[1])
        return v

    KW = 1024 // 4
    R0, R1, R2, R3, R4 = 0, 40 * KW, 80 * KW, 120 * KW, 168 * KW
    xnT = a16(R0, [32, 640])
    ogT = a16(R1, [32, 640])
    mgT = a16(R2, [32, 640])
    xn2T = mgT
    x1 = a32(R0, [5, 4096])
    wring = [a16(R3 + i * 16 * KW, [8192]) for i in range(3)]
    wstate = {"i": 0}

    sem_names = ["w0", "w1", "w2", "x", "c", "st", "so", "og", "y", "cv", "m"]
    esem = {e: es.enter_context(nc.semaphore("se_" + e)) for e in ("pe", "act", "dve", "pool")}
    dsem = {s: es.enter_context(nc.semaphore("sd_" + s)) for s in Prog.all_slots()}

    uid = [0]

    def tok(prefix):
        uid[0] += 1
        return "%s#%d" % (prefix, uid[0])

    open_groups = {}

    def mm(out, lhsT, rhs, start, stop, reads, writes):
        o = P.op("pe", lambda e: e.matmul(out, lhsT, rhs, start=start, stop=stop), reads, writes)
        key = writes[0]
        if start:
            assert key not in open_groups, ("accumulation group still open on", key)
            open_groups[key] = []
        open_groups[key].append(o)
        if stop:
            for g in open_groups.pop(key):
                if g is not o:
                    g.redirect = o

    def tr(out, in_, idn, reads, writes):
        P.op("pe", lambda e: e.transpose(out, in_, idn), reads, writes)

    def act(out, in_, func, reads, writes, scale=1.0, bias=0.0, accum=None):
        if accum is None:
            P.op("act", lambda e: e.activation(out, in_, func, bias=bias, scale=scale), reads, writes)
        else:
            P.op("act", lambda e: e.activation(out, in_, func, bias=bias, scale=scale, accum_out=accum),
                 reads, writes)

    def tt(eng, out, in0, in1, op, reads, writes):
        eng = "dve" if eng == "pool" else eng
        P.op(eng, lambda e: e.tensor_tensor(out, in0, in1, op), reads, writes)

    def ts(eng, out, in0, s1, s2, op0, op1, reads, writes):
        eng = "dve" if eng == "pool" else eng
        if s2 is None:
            P.op(eng, lambda e: e.tensor_scalar(out, in0, s1, None, op0), reads, writes)
        else:
            P.op(eng, lambda e: e.tensor_scalar(out, in0, s1, s2, op0, op1), reads, writes)

    def stt(eng, out, in0, sc, in1, op0, op1, reads, writes):
        P.op(eng, lambda e: e.scalar_tensor_tensor(out, in0, sc, in1, op0, op1), reads, writes)

    def cp(eng, out, in_, reads, writes):
        eng = "act" if eng == "pool" else eng
        if eng == "act":
            P.op("act", lambda e: e.copy(out, in_), reads, writes)
        else:
            P.op(eng, lambda e: e.tensor_copy(out, in_), reads, writes)

    def dma(slot, out, in_, reads, writes, eng="sp"):
        P.dma(eng, slot, lambda e: e.dma_start(out=out, in_=in_), reads, writes)

    def load_w(src, kcn, cols):
        i = wstate["i"] % 3
        wstate["i"] += 1
        buf = wring[i][:, 0:kcn * cols].rearrange("p (k c) -> p k c", k=kcn)
        t = "wring%d" % i
        P.dma("pool", "w%d" % i,
              lambda e: e.dma_start(out=buf, in_=src.rearrange("(k p) c -> p k c", p=128)),
              (), (t,))
        return buf, t

    def rstd_from_ss(ss, n, reads_writes_tok):
        act(ss, ss, AF.Sqrt, (reads_writes_tok,), (reads_writes_tok,), scale=1.0 / n, bias=EPS)
        P.op("dve", lambda e: e.reciprocal(ss, ss), (reads_writes_tok,), (reads_writes_tok,))

    def load_consts():
        for dst, src, name in ((ident_f, k_ident, "ident_f"), (dm, k_dm, "dm"), (dm4, k_dm4, "dm4"),
                               (dec, k_dec, "dec"), (tri16, k_tri16, "tri16"), (blk16, k_blk16, "blk16"),
                               (tri4, k_tri4, "tri4"), (blk4, k_blk4, "blk4"), (cmT, k_cmT, "cmT")):
            dma("c", dst[:], src, (), (name,))
        dma("c", gmixT[:], g_mix, (), ("gmixT",))
        dma("c", gffnT[:], g_ffn, (), ("gffnT",))
        dma("c", cwb[:], cwb_d.rearrange("p (a b) -> p a b", b=4), (), ("cw",))
        P.dma("pool", "m", lambda e: e.dma_start(out=ident_b[:], in_=k_ident), (), ("ident_b",))
        for dst_, src_, nm_ in ((tri16b, k_tri16, "tri16b"), (blk16b, k_blk16, "blk16b"), (tri4b, k_tri4, "tri4b"),
                                (blk4b, k_blk4, "blk4b"), (cmTb, k_cmT64, "cmTb")):
            P.dma("pool", "m", lambda e, dst_=dst_, src_=src_: e.dma_start(out=dst_[:], in_=src_), (), (nm_,))
        P.dma("pool", "m", lambda e: e.dma_start(out=cm16[:], in_=k_cm16.rearrange("p (a b) -> p a b", a=8)),
              (), ("cm16",))
        P.dma("pool", "m", lambda e: e.dma_start(out=cm4[:], in_=k_cm4.rearrange("p (a b) -> p a b", a=16)),
              (), ("cm4",))
        P.op("dve", lambda e: e.memset(carry[:], 0.0), (), ("carry",))

    def norm_phase(tiles, dstT, gT, gtok, src_x1=False, wbase=R1, dtok="actT"):
        xt = [a32(wbase + i * 4096, [4096]) for i in range(2)]
        xb = [a16(wbase + 8192 + i * 2048, [4096]) for i in range(2)]
        for li, tt_ in enumerate(tiles):
            b = li % 2
            if src_x1:
                src = x1[:, tt_, :]
                srct = "x1_%d" % tt_
            else:
                src = xt[b]
                srct = "xt%d" % b
                dma("x", xt[b], xa[tt_ * 128:(tt_ + 1) * 128, :], (), (srct,))
            ss = small[:, 2 * b:2 * b + 1]
            sst = "nss%d" % b
            xbt = "xb%d" % b
            act(xb[b], src, AF.Square, (srct,), (xbt, sst), accum=ss)
            rstd_from_ss(ss, D, sst)
            ts("dve", xb[b], src, ss, None, ALU.mult, None, (srct, sst), (xbt,))
            for k4 in range(8):
                pt = psum[2]
                ptk = "ps2"
                pv = pt[:, (k4 % 2) * 256:(k4 % 2) * 256 + 256].bitcast(BF16).rearrange("p (a b) -> p a b", a=4)
                for q in range(4):
                    kc = k4 * 4 + q
                    tr(pv[:, q, :], xb[b][:, kc * 128:(kc + 1) * 128], ident_b[:], (xbt, "ident_b"), (ptk,))
                for q in range(4):
                    kc = k4 * 4 + q
                    eng = "act" if q % 2 == 0 else "dve"
                    o = dstT[:, kc, li * 128:(li + 1) * 128]
                    if eng == "act":
                        act(o, pv[:, q, :], AF.Copy, (ptk, gtok), (dtok,), scale=gT[:, kc:kc + 1])
                    else:
                        ts("dve", o, pv[:, q, :], gT[:, kc:kc + 1], None, ALU.mult, None, (ptk, gtok), (dtok,))

    def proj(srcT, srct, ntile, wsrc, ncols, consume, kcn=32, koff=0):
        wb, wt = load_w(wsrc, kcn, ncols)
        for ti in range(ntile):
            pb = ti % 2
            ps = psum[pb][:, 0:ncols]
            for kc in range(kcn):
                mm(ps, srcT[:, koff + kc, ti * 128:(ti + 1) * 128], wb[:, kc, :], kc == 0, kc == kcn - 1,
                   (srct, wt), ("ps%d" % pb,))
            consume(ti, ps, "ps%d" % pb)

    def ret_unit(h, tiles, outputs, first, last_store_out, tok_off):
        nt = len(tiles)
        W = R1
        st_q = a32(W, [nt, 256]); W += nt * 256
        st_k = a32(W, [nt, 256]); W += nt * 256
        sg = a32(W, [nt, 256]); W += nt * 256
        st_v = a16(W, [nt, 256]); W += nt * 128
        tmp = [a32(W + i * 128, [128]) for i in range(4)]; W += 512
        q_bf = a16(W, [256]); W += 128
        k_bf = a16(W, [256]); W += 128
        qd_bf = a16(W, [256]); W += 128
        kd_bf = a16(W, [256]); W += 128
        qkT = a16(W, [6, 128]); W += 384
        scT = a16(W, [128]); W += 64
        S = a32(W, [2, 256]); W += 512
        S_bf = a16(W, [2, 256]); W += 256
        og_bf = a16(W, [256]); W += 128
        ogTt = a16(W, [2, 128]); W += 128
        gsl = a32(W, [256]); W += 256
        cs = a32(W, [nt, 128]); W += nt * 128
        sn = a32(W, [nt, 128]); W += nt * 128
        kdm = a16(W, [256]); W += 128
        if 16 in tiles:
            S0 = a32(W, [2, 8, 256]); W += 4096
            S0b = a16(W, [2, 8, 256]); W += 2048
            Sn = a32(W, [2, 8, 256]); W += 4096
            qdm = a16(W, [2, 8, 128]); W += 1024
        osum = a32(W, [256]); W += 256
        assert W <= R3, W
        u = "r_"

        for li, tg in enumerate(tiles):
            dma("c", cs[:, li, :], cosT[tg], (), (u + "cs",))
            dma("c", sn[:, li, :], sinT[tg], (), (u + "sn",))
        if outputs:
            dma("c", gsl, ret_g[0:1, h * 256:(h + 1) * 256].to_broadcast([128, 256]), (), (u + "gsl",))

        def ev_q(ti, ps, pt):
            cp("act", st_q[:, ti, :], ps, (pt,), (u + "stq%d" % ti,))

        def ev_k(ti, ps, pt):
            act(st_k[:, ti, :], ps, AF.Copy, (pt,), (u + "stk%d" % ti,), scale=1.0 / 16.0)

        def ev_v(ti, ps, pt):
            cp("act", st_v[:, ti, :], ps, (pt,), (u + "stv%d" % ti,))

        def ev_g(ti, ps, pt):
            act(sg[:, ti, :], ps, AF.Silu, (pt,), (u + "sg%d" % ti,))
            tt("pool", sg[:, ti, :], sg[:, ti, :], gsl, ALU.mult, (u + "sg%d" % ti, u + "gsl"), (u + "sg%d" % ti,))

        if outputs:
            proj(xnT, "actT", nt, w_in[:, C_QR + h * 256:C_QR + (h + 1) * 256], 256, ev_q)
        proj(xnT, "actT", nt, w_in[:, C_KR + h * 256:C_KR + (h + 1) * 256], 256, ev_k)
        proj(xnT, "actT", nt, w_in[:, C_VR + h * 256:C_VR + (h + 1) * 256], 256, ev_v)
        if outputs:
            proj(xnT, "actT", nt, w_in[:, C_GR + h * 256:C_GR + (h + 1) * 256], 256, ev_g)

        St = u + "S"
        if first:
            P.op("dve", lambda e: e.memset(S, 0.0), (), (St,))
        else:
            dma("st", S, sr_scr[h].rearrange("(c p) v -> p c v", p=128), ("sr_scr%d" % h,), (St,))
        if outputs:
            cp("pool", S_bf, S, (St,), (u + "Sbf",))

        def rope(st, ti, dst, scale, name):
            x1_, x2_ = st[:, ti, 0:128], st[:, ti, 128:256]
            c_, s_ = cs[:, ti, :], sn[:, ti, :]
            rd = (name + "%d" % ti, u + "cs", u + "sn")
            tt("dve", tmp[0], x1_, c_, ALU.mult, rd, (u + "t0",))
            tt("pool", tmp[1], x2_, s_, ALU.mult, rd, (u + "t1",))
            tt("pool", tmp[2], x1_, s_, ALU.mult, rd, (u + "t2",))
            tt("dve", tmp[3], x2_, c_, ALU.mult, rd, (u + "t3",))
            tt("dve", dst[:, 0:128], tmp[0], tmp[1], ALU.subtract, (u + "t0", u + "t1"), (u + dst_name[id(dst)],))
            tt("pool", dst[:, 128:256], tmp[2], tmp[3], ALU.add, (u + "t2", u + "t3"), (u + dst_name[id(dst)],))

        dst_name = {id(q_bf): "qbf", id(k_bf): "kbf"}

        for ti, tg in enumerate(tiles):
            sample = (tg == 16)
            mask = dm4 if sample else dm
            mtok = "dm4" if sample else "dm"
            qcol = 16 if sample else 0
            kcol = 24 if sample else 8
            rope(st_k, ti, k_bf, 1.0 / 16.0, u + "stk")
            ts("dve", kd_bf, k_bf, dec[:, kcol + h:kcol + h + 1], None, ALU.mult, None, (u + "kbf", "dec"), (u + "kdbf",))
            if outputs:
                rope(st_q, ti, q_bf, 1.0, u + "stq")
                ts("pool", qd_bf, q_bf, dec[:, qcol + h:qcol + h + 1], None, ALU.mult, None, (u + "qbf", "dec"),
                   (u + "qdbf",))
                pv = psum[2][:, 0:384].bitcast(BF16).rearrange("p (a b) -> p a b", a=6)
                for c in range(2):
                    tr(pv[:, c, :], q_bf[:, c * 128:(c + 1) * 128], ident_b[:], (u + "qbf", "ident_b"), ("ps2",))
                    tr(pv[:, 2 + c, :], k_bf[:, c * 128:(c + 1) * 128], ident_b[:], (u + "kbf", "ident_b"), ("ps2",))
                    tr(pv[:, 4 + c, :], qd_bf[:, c * 128:(c + 1) * 128], ident_b[:], (u + "qdbf", "ident_b"), ("ps2",))
                cp("act", qkT, pv, ("ps2",), (u + "qkT",))
                sc = psum[3][:, 0:128]
                for c in range(2):
                    mm(sc, qkT[:, 2 + c, :], qkT[:, c, :], c == 0, c == 1, (u + "qkT",), ("ps3",))
                tt("dve", scT, sc, mask[:, h, :], ALU.mult, ("ps3", mtok), (u + "scT",))
                o = psum[4][:, 0:256]
                vtk = u + "stv%d" % ti
                if not sample:
                    mm(o, scT, st_v[:, ti, :], True, False, (u + "scT", vtk), ("ps4",))
                    for c in range(2):
                        mm(o, qkT[:, 4 + c, :], S_bf[:, c, :], False, c == 1, (u + "qkT", u + "Sbf"), ("ps4",))
            if not sample:
                kv = psum[5][:, 0:512].rearrange("p (c v) -> p c v", c=2)
                for c in range(2):
                    mm(kv[:, c, :], kd_bf[:, c * 128:(c + 1) * 128], st_v[:, ti, :], True, True,
                       (u + "kdbf", u + "stv%d" % ti), ("ps5",))
                sdec = float(np.float32(GAM[h]) ** 128)
                stt("dve", S, S, sdec, kv, ALU.mult, ALU.add, (St, "ps5"), (St,))
                if outputs:
                    cp("pool", S_bf, S, (St,), (u + "Sbf",))
            else:
                sdec = float(np.float32(GAM[h]) ** 4)
                for half in range(2):
                    s0t = u + "S0"
                    for c_ in range(2):
                        dma("st", S0[:, c_], sret[half * 8:(half + 1) * 8, h, c_ * 128:(c_ + 1) * 128, :]
                            .rearrange("s p v -> p s v"), (), (s0t,))
                    cp("pool", S0b, S0, (s0t,), (u + "S0b",))
                    for c_ in range(2):
                        P.op("dve", lambda e, c_=c_, half=half: e.tensor_tensor(
                            qdm[:, c_], qkT[:, 4 + c_, :].unsqueeze(1).to_broadcast([128, 8, 128]),
                            cm4[:, half * 8:(half + 1) * 8, :], ALU.mult),
                            (u + "qkT", "cm4"), (u + "qdm",))
                    oh_ = psum[4][:, half * 256:(half + 1) * 256]
                    if half == 0:
                        mm(oh_, scT, st_v[:, ti, :], True, False, (u + "scT", u + "stv%d" % ti), ("ps4",))
                    for s in range(8):
                        for c in range(2):
                            mm(oh_, qdm[:, c, s, :], S0b[:, c, s, :], (half == 1 and s == 0 and c == 0),
                               (s == 7 and c == 1), (u + "qdm", u + "S0b"), ("ps4",))
                    for s in range(8):
                        sg_ = half * 8 + s
                        ts("pool", kdm, kd_bf, cmT[:, 8 + sg_:9 + sg_], None, ALU.mult, None, (u + "kdbf", "cmT"),
                           (u + "kdm",))
                        kv = psum[5][:, 0:512].rearrange("p (c v) -> p c v", c=2)
                        for c in range(2):
                            mm(kv[:, c, :], kdm[:, c * 128:(c + 1) * 128], st_v[:, ti, :], True, True,
                               (u + "kdm", u + "stv%d" % ti), ("ps5",))
                        stt("dve", Sn[:, :, s, :], S0[:, :, s, :], sdec, kv, ALU.mult, ALU.add, (s0t, "ps5"),
                            (u + "Sn",))
                    for c_ in range(2):
                        dma("so", rets_o[half * 8:(half + 1) * 8, h, c_ * 128:(c_ + 1) * 128, :]
                            .rearrange("s p v -> p s v"), Sn[:, c_], (u + "Sn",), ())
            if outputs:
                ss = small[:, 8:9]
                cp("act", osum, psum[4][:, 0:256], ("ps4",), (u + "osum",))
                if sample:
                    tt("dve", osum, psum[4][:, 256:512], osum, ALU.add, ("ps4", u + "osum"), (u + "osum",))
                osrc, osrct = osum, u + "osum"
                act(og_bf, osrc, AF.Square, (osrct,), (u + "ogbf", u + "ss"), accum=ss)
                rstd_from_ss(ss, 256, u + "ss")
                stt("dve", og_bf, osrc, ss, sg[:, ti, :], ALU.mult, ALU.mult, (osrct, u + "ss", u + "sg%d" % ti),
                    (u + "ogbf",))
                pv2 = psum[2][:, 0:128].bitcast(BF16).rearrange("p (a b) -> p a b", a=2)
                for c in range(2):
                    tr(pv2[:, c, :], og_bf[:, c * 128:(c + 1) * 128], ident_b[:], (u + "ogbf", "ident_b"), ("ps2",))
                cp("act", ogTt, pv2, ("ps2",), (u + "ogT",))
                t0 = tok_off + ti * 128
                dma("og", og_scr[h * 2:h * 2 + 2, :, t0:t0 + 128].rearrange("c p t -> p c t"), ogTt,
                    (u + "ogT",), ("og_scr",))
        dma("st", sr_scr[h].rearrange("(c p) v -> p c v", p=128), S, (St,), ("sr_scr%d" % h,))
        if last_store_out:
            dma("so", retp_o[h].rearrange("(c p) v -> p c v", p=128), S, (St,), ())

    def hg_unit(un, tiles, outputs, first, last_store_out, tok_off):
        nt = len(tiles)
        W = R1
        sgf = a32(W, [nt, 256]); W += nt * 256
        lgf = a32(W, [nt, 256]); W += nt * 256
        kh = a32(W, [nt, 256]); W += nt * 256
        if outputs:
            st_q = a32(W, [nt, 256]); W += nt * 256
            sg = a32(W, [nt, 256]); W += nt * 256
        vh = a16(W, [nt, 256]); W += nt * 128
        lb = a32(W, [256]); W += 256
        oml = a32(W, [256]); W += 256
        l1 = a32(W, [256]); W += 256
        gsl = a32(W, [256]); W += 256
        b_sb = a32(W, [256]); W += 256
        eb = a32(W, [256]); W += 256
        enb = a32(W, [256]); W += 256
        lh = a16(W, [256]); W += 128
        ll = a16(W, [256]); W += 128
        dk = a32(W, [256]); W += 256
        qt_bf = a16(W, [256]); W += 128
        kt_bf = a16(W, [256]); W += 128
        kd_bf = a16(W, [256]); W += 128
        qkT = a16(W, [4, 128]); W += 256
        ebT = a32(W, [2, 32]); W += 64
        scT = a16(W, [128]); W += 64
        kdm = a16(W, [8, 128]); W += 512
        Sb = [a32(W + i * 1152, [9, 128]) for i in range(2)]; W += 2304
        Sbb = a16(W, [8, 128]); W += 512
        qTm = a16(W, [8, 128]); W += 512
        og_bf = a16(W, [256]); W += 128
        ogTt = a16(W, [2, 128]); W += 128
        if 16 in tiles:
            S0 = a32(W, [16, 128]); W += 2048
            S0b = a16(W, [16, 128]); W += 1024
            Sn = a32(W, [16, 128]); W += 2048
        osum = a32(W, [128]); W += 128
        assert W <= R3, W
        u = "h_"
        c0 = un * 256

        dma("c", lb, lbl[0:1, c0:c0 + 256].to_broadcast([128, 256]), (), (u + "lb",))
        dma("c", l1, lbl[1:2, c0:c0 + 256].to_broadcast([128, 256]), (), (u + "l1",))
        tt("dve", lb, lb, l1, ALU.subtract, (u + "lb", u + "l1"), (u + "lb",))
        act(lb, lb, AF.Sigmoid, (u + "lb",), (u + "lb",))
        ts("dve", oml, lb, -1.0, 1.0, ALU.mult, ALU.add, (u + "lb",), (u + "oml",))
        if outputs:
            dma("c", gsl, hg_g[0:1, c0:c0 + 256].to_broadcast([128, 256]), (), (u + "gsl",))

        def ev_f(ti, ps, pt):
            t = u + "f%d" % ti
            act(sgf[:, ti, :], ps, AF.Sigmoid, (pt,), (t,))
            tt("dve", sgf[:, ti, :], sgf[:, ti, :], oml, ALU.mult, (t, u + "oml"), (t,))
            tt("dve", sgf[:, ti, :], sgf[:, ti, :], lb, ALU.add, (t, u + "lb"), (t,))
            act(lgf[:, ti, :], sgf[:, ti, :], AF.Ln, (t,), (u + "lgf%d" % ti,))
            ts("pool", kh[:, ti, :], sgf[:, ti, :], -1.0, 1.0, ALU.mult, ALU.add, (t,), (u + "kh%d" % ti,))

        def ev_q(ti, ps, pt):
            cp("dve", st_q[:, ti, :], ps, (pt,), (u + "stq%d" % ti,))

        def ev_i(ti, ps, pt):
            act(vh[:, ti, :], ps, AF.Silu, (pt,), (u + "vh%d" % ti,))

        def ev_g(ti, ps, pt):
            act(sg[:, ti, :], ps, AF.Silu, (pt,), (u + "sg%d" % ti,))
            tt("pool", sg[:, ti, :], sg[:, ti, :], gsl, ALU.mult, (u + "sg%d" % ti, u + "gsl"), (u + "sg%d" % ti,))

        proj(xnT, "actT", nt, w_in[:, C_FH + c0:C_FH + c0 + 256], 256, ev_f)
        if outputs:
            proj(xnT, "actT", nt, w_in[:, C_QH + c0:C_QH + c0 + 256], 256, ev_q)
        proj(xnT, "actT", nt, w_in[:, C_IH + c0:C_IH + c0 + 256], 256, ev_i)
        if outputs:
            proj(xnT, "actT", nt, w_in[:, C_GH + c0:C_GH + c0 + 256], 256, ev_g)

        for hh in range(2):
            hd = un * 2 + hh
            if first:
                P.op("dve", lambda e, hh=hh: e.memset(Sb[hh][:, 0, :], 0.0), (), (u + "S%d" % hh,))
            else:
                dma("st", Sb[hh][:, 0, :], sh_scr[hd], ("sh_scr%d" % hd,), (u + "S%d" % hh,))

        import os
        hgdbg = int(os.environ.get("HGDBG", "0"))
        for ti, tg in enumerate(tiles):
            if hgdbg == 1:
                break
            sample = (tg == 16)
            tri, blk = (tri4, blk4) if sample else (tri16, blk16)
            trit, blkt = ("tri4", "blk4") if sample else ("tri16", "blk16")
            ncm = 16 if sample else 8
            step = 4 if sample else 16
            trib, blkb = (tri4b, blk4b) if sample else (tri16b, blk16b)
            tribt, blkbt = ("tri4b", "blk4b") if sample else ("tri16b", "blk16b")
            cs_ = psum[3][:, 0:512].rearrange("p (a b) -> p a b", a=2)
            lt = u + "lgf%d" % ti
            cp("pool", lh, lgf[:, ti, :], (lt,), (u + "lh",))
            tt("dve", ll, lgf[:, ti, :], lh, ALU.subtract, (lt, u + "lh"), (u + "ll",))
            if hgdbg == 19:
                continue
            mm(cs_[:, 0, :], trib[:], lh, True, False, (tribt, u + "lh"), ("ps3",))
            mm(cs_[:, 0, :], trib[:], ll, False, True, (tribt, u + "ll"), ("ps3",))
            mm(cs_[:, 1, :], blkb[:], lh, True, False, (blkbt, u + "lh"), ("ps3",))
            mm(cs_[:, 1, :], blkb[:], ll, False, True, (blkbt, u + "ll"), ("ps3",))
            if hgdbg == 20:
                continue
            cp("dve", b_sb, cs_[:, 0, :], ("ps3",), (u + "b",))
            act(enb, b_sb, AF.Exp, (u + "b",), (u + "enb",), scale=-1.0)
            if hgdbg == 21:
                continue
            tt("dve", dk, cs_[:, 1, :], b_sb, ALU.subtract, ("ps3", u + "b"), (u + "dk",))
            act(dk, dk, AF.Exp, (u + "dk",), (u + "dk",))
            tt("pool", kd_bf, kh[:, ti, :], dk, ALU.mult, (u + "kh%d" % ti, u + "dk"), (u + "kdbf",))
            if outputs:
                act(eb, b_sb, AF.Exp, (u + "b",), (u + "eb",))
                tt("dve", qt_bf, st_q[:, ti, :], eb, ALU.mult, (u + "stq%d" % ti, u + "eb"), (u + "qtbf",))
                tt("pool", kt_bf, kh[:, ti, :], enb, ALU.mult, (u + "kh%d" % ti, u + "enb"), (u + "ktbf",))
                pv = psum[2][:, 0:256].bitcast(BF16).rearrange("p (a b) -> p a b", a=4)
                for hh in range(2):
                    tr(pv[:, hh, :], qt_bf[:, hh * 128:(hh + 1) * 128], ident_b[:], (u + "qtbf", "ident_b"), ("ps2",))
                    tr(pv[:, 2 + hh, :], kt_bf[:, hh * 128:(hh + 1) * 128], ident_b[:], (u + "ktbf", "ident_b"),
                       ("ps2",))
                cp("act", qkT, pv, ("ps2",), (u + "qkT",))
            if hgdbg == 2:
                continue
            pe_ = psum[4][:, 0:64].rearrange("p (a b) -> p a b", a=2)
            cmo = 32 if sample else 0
            for hh in range(2):
                mm(pe_[:, hh, :], lh[:, hh * 128:(hh + 1) * 128], cmTb[:, cmo:cmo + 32], True, False,
                   (u + "lh", "cmTb"), ("ps4",))
                mm(pe_[:, hh, :], ll[:, hh * 128:(hh + 1) * 128], cmTb[:, cmo:cmo + 32], False, True,
                   (u + "ll", "cmTb"), ("ps4",))
            cp("dve", ebT[:, :, 0:ncm], pe_[:, :, 0:ncm], ("ps4",), (u + "ebT",))
            act(ebT[:, :, 0:ncm], ebT[:, :, 0:ncm], AF.Exp, (u + "ebT",), (u + "ebT",))
            if hgdbg == 3:
                continue
            for hh in range(2):
                hd = un * 2 + hh
                St = u + "S%d" % hh
                vsl = vh[:, ti, hh * 128:(hh + 1) * 128]
                vt = u + "vh%d" % ti
                o = psum[4][:, 256 + hh * 128:256 + (hh + 1) * 128]
                if outputs:
                    sc = psum[5][:, 0:128]
                    mm(sc, qkT[:, 2 + hh, :], qkT[:, hh, :], True, True, (u + "qkT",), ("ps5",))
                    tt("dve", scT, sc, tri[:], ALU.mult, ("ps5", trit), (u + "scT",))
                if not sample:
                    P.op("dve", lambda e, hh=hh: e.tensor_tensor(
                        kdm, kd_bf[:, hh * 128:(hh + 1) * 128].unsqueeze(1).to_broadcast([128, 8, 128]),
                        cmT[:, 0:8].unsqueeze(2).to_broadcast([128, 8, 128]), ALU.mult),
                        (u + "kdbf", "cmT"), (u + "kdm",))
                    for c in range(8):
                        mm(kvview(c), kdm[:, c, :], vsl, True, True, (u + "kdm", vt), ("pskv%d" % (c // 4),))
                    for c in range(8):
                        if hgdbg == 4:
                            break
                        stt("dve", Sb[hh][:, c + 1, :], Sb[hh][:, c, :], ebT[:, hh, c:c + 1], kvview(c),
                            ALU.mult, ALU.add, (St, "pskv%d" % (c // 4), u + "ebT"), (St,))
                    if outputs:
                        cp("pool", Sbb, Sb[hh][:, 0:8, :], (St,), (u + "Sbb",))
                        P.op("dve", lambda e, hh=hh: e.tensor_tensor(
                            qTm, qkT[:, hh, :].unsqueeze(1).to_broadcast([128, 8, 128]), cm16[:], ALU.mult),
                            (u + "qkT", "cm16"), (u + "qTm",))
                        mm(o, scT, vsl, True, False, (u + "scT", vt), ("ps4",))
                        for c in range(8):
                            mm(o, qTm[:, c, :], Sbb[:, c, :], False, c == 7, (u + "qTm", u + "Sbb"), ("ps4",))
                    cp("act", Sb[hh][:, 0, :], Sb[hh][:, 8, :], (St,), (St,))
                else:
                    s0t = u + "S0"
                    dma("st", S0, shg[:, hd].rearrange("s k v -> k s v"), (), (s0t,))
                    cp("pool", S0b, S0, (s0t,), (u + "S0b",))
                    o2 = psum[5][:, 256 + hh * 128:256 + (hh + 1) * 128]
                    mm(o, scT, vsl, True, False, (u + "scT", vt), ("ps4",))
                    for half in range(2):
                        P.op("dve", lambda e, hh=hh, half=half: e.tensor_tensor(
                            qTm, qkT[:, hh, :].unsqueeze(1).to_broadcast([128, 8, 128]),
                            cm4[:, half * 8:(half + 1) * 8, :], ALU.mult),
                            (u + "qkT", "cm4"), (u + "qTm",))
                        for s in range(8):
                            if half == 0:
                                mm(o, qTm[:, s, :], S0b[:, s, :], False, s == 7, (u + "qTm", u + "S0b"), ("ps4",))
                            else:
                                mm(o2, qTm[:, s, :], S0b[:, 8 + s, :], s == 0, s == 7, (u + "qTm", u + "S0b"),
                                   ("ps5",))
                        P.op("dve", lambda e, hh=hh, half=half: e.tensor_tensor(
                            kdm, kd_bf[:, hh * 128:(hh + 1) * 128].unsqueeze(1).to_broadcast([128, 8, 128]),
                            cmT[:, 8 + half * 8:16 + half * 8].unsqueeze(2).to_broadcast([128, 8, 128]), ALU.mult),
                            (u + "kdbf", "cmT"), (u + "kdm",))
                        for s in range(8):
                            mm(kvview(s), kdm[:, s, :], vsl, True, True, (u + "kdm", vt), ("pskv%d" % (s // 4),))
                        for s in range(8):
                            sq = half * 8 + s
                            stt("dve", Sn[:, sq, :], S0[:, sq, :], ebT[:, hh, sq:sq + 1], kvview(s),
                                ALU.mult, ALU.add, (s0t, "pskv%d" % (s // 4), u + "ebT"), (u + "Sn",))
                    dma("so", hgs_o[:, hd].rearrange("s k v -> k s v"), Sn, (u + "Sn",), ())
                if outputs:
                    ss = small[:, 10 + hh:11 + hh]
                    sst = u + "ss%d" % hh
                    osl = og_bf[:, hh * 128:(hh + 1) * 128]
                    cp("act", osum, o, ("ps4",), (u + "osum",))
                    if sample:
                        tt("dve", osum, o2, osum, ALU.add, ("ps5", u + "osum"), (u + "osum",))
                    osrc, osrct = osum, u + "osum"
                    act(osl, osrc, AF.Square, (osrct,), (u + "ogbf", sst), accum=ss)
                    rstd_from_ss(ss, 128, sst)
                    stt("dve", osl, osrc, ss, sg[:, ti, hh * 128:(hh + 1) * 128], ALU.mult, ALU.mult,
                        (osrct, sst, u + "sg%d" % ti), (u + "ogbf",))
            if outputs:
                pv2 = psum[2][:, 0:128].bitcast(BF16).rearrange("p (a b) -> p a b", a=2)
                for c in range(2):
                    tr(pv2[:, c, :], og_bf[:, c * 128:(c + 1) * 128], ident_b[:], (u + "ogbf", "ident_b"), ("ps2",))
                cp("act", ogTt, pv2, ("ps2",), (u + "ogT",))
                t0 = tok_off + ti * 128
                ch = 16 + un * 2
                dma("og", og_scr[ch:ch + 2, :, t0:t0 + 128].rearrange("c p t -> p c t"), ogTt,
                    (u + "ogT",), ("og_scr",))
        for hh in range(2):
            hd = un * 2 + hh
            St = u + "S%d" % hh
            dma("st", sh_scr[hd], Sb[hh][:, 0, :], (St,), ("sh_scr%d" % hd,))
            if last_store_out:
                dma("so", hgp_o[hd], Sb[hh][:, 0, :], (St,), ())

    def merge_phase(tok_off):
        W = R4
        s1 = a32(W, [5, 256]); W += 1280
        mg = a16(W, [5, 256]); W += 640
        assert W <= AW
        dma("og", ogT, og_scr[:, :, tok_off:tok_off + 640].rearrange("c p t -> p c t"), ("og_scr",), ("ogT",))
        for j in range(16):
            def ev_gr(ti, ps, pt):
                act(s1[:, ti, :], ps, AF.Sigmoid, (pt,), ("s1_%d" % ti,))

            def ev_pr(ti, ps, pt):
                tt("dve", s1[:, ti, :], ps, s1[:, ti, :], ALU.mult, (pt, "s1_%d" % ti), ("s1_%d" % ti,))

            m2 = a32(R4 + 1920, [5, 256])

            def ev_gh(ti, ps, pt):
                act(m2[:, ti, :], ps, AF.Sigmoid, (pt,), ("m2_%d" % ti,))

            def ev_ph(ti, ps, pt, j=j):
                tt("dve", m2[:, ti, :], ps, m2[:, ti, :], ALU.mult, (pt, "m2_%d" % ti), ("m2_%d" % ti,))
                tt("pool", mg[:, ti, :], m2[:, ti, :], s1[:, ti, :], ALU.add, ("m2_%d" % ti, "s1_%d" % ti),
                   ("mg_%d" % ti,))
                pv = psum[2][:, 0:128].bitcast(BF16).rearrange("p (a b) -> p a b", a=2)
                for c in range(2):
                    tr(pv[:, c, :], mg[:, ti, c * 128:(c + 1) * 128], ident_b[:], ("mg_%d" % ti, "ident_b"), ("ps2",))
                cp("act", mgT[:, j * 2:j * 2 + 2, ti * 128:(ti + 1) * 128], pv, ("ps2",), ("mgT",))

            proj(xnT, "actT", 5, w_in[:, C_GATE_R + j * 256:C_GATE_R + (j + 1) * 256], 256, ev_gr)
            proj(ogT, "ogT", 5, w_br_ret[:, j * 256:(j + 1) * 256], 256, ev_pr, kcn=16, koff=0)
            proj(xnT, "actT", 5, w_in[:, C_GATE_H + j * 256:C_GATE_H + (j + 1) * 256], 256, ev_gh)
            proj(ogT, "ogT", 5, w_br_hg[:, j * 256:(j + 1) * 256], 256, ev_ph, kcn=16, koff=16)

    def wout_phase(tiles):
        for li, tg in enumerate(tiles):
            dma("x", x1[:, li, :], xa[tg * 128:(tg + 1) * 128, :], (), ("x1_%d" % li,))
        for j in range(16):
            def ev(ti, ps, pt, j=j):
                tt("dve", x1[:, ti, j * 256:(j + 1) * 256], ps, x1[:, ti, j * 256:(j + 1) * 256], ALU.add,
                   (pt, "x1_%d" % ti), ("x1_%d" % ti,))
            proj(mgT, "mgT", 5, w_out[:, j * 256:(j + 1) * 256], 256, ev)

    def ffn_phase(grp):
        W = R4
        ue = a32(W, [644]); W += 644
        cc = a32(W, [640]); W += 640
        hT = a16(W, [2, 640]); W += 640
        ues = a32(W, [16, 6]); W += 96
        ccs = a32(W, [16, 4]); W += 64
        ctp = a32(W, [34]); W += 34
        W += (-W) % 2
        scv = a32(W, [256]); W += 256
        ct = a32(W, [256]); W += 256
        scv3 = [a16(W + i * 128, [256]) for i in range(3)]; W += 384
        scvr = a32(W, [256]); W += 256
        ctp3 = [a16(W + i * 17, [34]) for i in range(3)]; W += 52
        ctpr = a32(W, [34]); W += 34

        def split3(src, parts, res, srct, dstt):
            cp("pool", parts[0], src, (srct,), (dstt,))
            tt("dve", res, src, parts[0], ALU.subtract, (srct, dstt), (dstt + "r",))
            cp("pool", parts[1], res, (dstt + "r",), (dstt,))
            tt("dve", res, res, parts[1], ALU.subtract, (dstt + "r", dstt), (dstt + "r",))
            cp("pool", parts[2], res, (dstt + "r",), (dstt,))
        assert W <= AW, W
        down_tiles = [1, 2, 3, 4] if grp == 0 else [0, 1, 2, 3, 4]
        for fb in range(43):
            f0 = fb * 256
            wg, wgt = load_w(w_gate[:, f0:f0 + 256], 32, 256)
            wu, wut = load_w(w_up[:, f0:f0 + 256], 32, 256)
            wd, wdt = load_w(w_down[f0:f0 + 256, :], 2, 4096)
            if grp == 1:
                dma("cv", scv[0:32, :], sconv[:, f0:f0 + 256], (), ("scv",))
                split3(scv[0:32, :], [t_[0:32, :] for t_ in scv3], scvr[0:32, :], "scv", "scv3")
            for sub in range(2):
                fc = fb * 2 + sub
                for hf in range(2):
                    ps = psum[hf][:, 0:320]
                    for kc in range(32):
                        mm(ps, wg[:, kc, sub * 128:(sub + 1) * 128], xn2T[:, kc, hf * 320:(hf + 1) * 320],
                           kc == 0, kc == 31, (wgt, "actT2"), ("ps%d" % hf,))
                    cp("act", ue[:, 2 + hf * 320:2 + (hf + 1) * 320], ps, ("ps%d" % hf,), ("ue",))
                cp("pool", ue[:, 0:2], carry[:, fc, :], ("carry",), ("ue",))
                w0, w1_, w2, bb = (cwb[:, fc, 0:1], cwb[:, fc, 1:2], cwb[:, fc, 2:3], cwb[:, fc, 3:4])
                act(cc, ue[:, 2:642], AF.Identity, ("ue", "cw"), ("cc",), scale=w2, bias=bb)
                stt("dve", cc, ue[:, 1:641], w1_, cc, ALU.mult, ALU.add, ("ue", "cw", "cc"), ("cc",))
                stt("dve", cc, ue[:, 0:640], w0, cc, ALU.mult, ALU.add, ("ue", "cw", "cc"), ("cc",))
                if grp == 0:
                    cp("pool", carry[:, fc, :], ue[:, 640:642], ("ue",), ("carry",))
                else:
                    pst = psum[3][:, 256:288]
                    for q3 in range(3):
                        mm(pst, scv3[q3][0:32, sub * 128:(sub + 1) * 128], ident_b[0:32, 0:32], q3 == 0, q3 == 2,
                           ("scv3", "ident_b"), ("ps3",))
                    cp("act", ues[:, :, 0:2], pst.rearrange("p (s j) -> p s j", j=2), ("ps3",), ("ues",))
                    cp("pool", ues[:, :, 2:6], ue[:, 514:578].rearrange("p (s j) -> p s j", j=4), ("ue",), ("ues",))
                    act(ccs, ues[:, :, 2:6], AF.Identity, ("ues", "cw"), ("ccs",), scale=w2, bias=bb)
                    stt("dve", ccs, ues[:, :, 1:5], w1_, ccs, ALU.mult, ALU.add, ("ues", "cw", "ccs"), ("ccs",))
                    stt("dve", ccs, ues[:, :, 0:4], w0, ccs, ALU.mult, ALU.add, ("ues", "cw", "ccs"), ("ccs",))
                    cp("pool", cc[:, 512:576].rearrange("p (s j) -> p s j", j=4), ccs, ("ccs", "cc"), ("cc",))
                    cp("pool", ctp[:, 0:32].rearrange("p (s j) -> p s j", j=2), ues[:, :, 4:6], ("ues",), ("ctp",))
                    cp("pool", ctp[:, 32:34], ue[:, 512:514], ("ue",), ("ctp",))
                    split3(ctp, ctp3, ctpr, "ctp", "ctp3")
                    pso = psum[3][0:34, 0:128]
                    for q3 in range(3):
                        mm(pso, ctp3[q3], ident_b[:], q3 == 0, q3 == 2, ("ctp3", "ident_b"), ("ps3",))
                    cp("act", ct[0:34, sub * 128:(sub + 1) * 128], pso, ("ps3",), ("ct",))
                act(cc, cc, AF.Silu, ("cc",), ("cc",))
                for hf in range(2):
                    ps = psum[hf][:, 0:320]
                    for kc in range(32):
                        mm(ps, wu[:, kc, sub * 128:(sub + 1) * 128], xn2T[:, kc, hf * 320:(hf + 1) * 320],
                           kc == 0, kc == 31, (wut, "actT2"), ("ps%d" % hf,))
                    tt("dve", hT[:, sub, hf * 320:(hf + 1) * 320], ps, cc[:, hf * 320:(hf + 1) * 320], ALU.mult,
                       ("ps%d" % hf, "cc"), ("hT",))
            if grp == 1:
                dma("cv", convs_o[:, f0:f0 + 256], ct[0:32, :], ("ct",), ())
                dma("cv", convp_o[:, f0:f0 + 256], ct[32:34, :], ("ct",), ())
            n = 0
            for ti in down_tiles:
                for cb in range(8):
                    pb = 4 + (n % 2)
                    n += 1
                    ps = psum[pb][:, 0:512]
                    for sub in range(2):
                        mm(ps, hT[:, sub, ti * 128:(ti + 1) * 128], wd[:, sub, cb * 512:(cb + 1) * 512],
                           sub == 0, sub == 1, ("hT", wdt), ("ps%d" % pb,))
                    tt("dve", x1[:, ti, cb * 512:(cb + 1) * 512], ps, x1[:, ti, cb * 512:(cb + 1) * 512], ALU.add,
                       ("ps%d" % pb, "x1_%d" % ti), ("x1_%d" % ti,))
        P.barrier()
        gfin = a32(R2, [4096])
        junk = a16(R2 + 4096, [4096])
        dma("c", gfin, g_fin.to_broadcast([128, D]), (), ("gfin",))
        for ti in down_tiles:
            ss = small[:, 20 + ti:21 + ti]
            sst = "fss%d" % ti
            act(junk, x1[:, ti, :], AF.Square, ("x1_%d" % ti,), ("junk", sst), accum=ss)
            rstd_from_ss(ss, D, sst)
            stt("dve", x1[:, ti, :], x1[:, ti, :], ss, gfin, ALU.mult, ALU.mult, ("x1_%d" % ti, sst, "gfin"),
                ("x1_%d" % ti,))
            if grp == 0:
                r0, nr = (ti - 1) * 128, 128
            elif ti < 4:
                r0, nr = 512 + ti * 128, 128
            else:
                r0, nr = 1024, 64
            dma("y", y_o[r0:r0 + nr, :], x1[0:nr, ti, :], ("x1_%d" % ti,), ())

    import os
    kstop = int(os.environ.get("KSTOP", "999"))
    phase_no = [0]

    def phase_ok():
        phase_no[0] += 1
        return phase_no[0] <= kstop

    load_consts()
    pre_tiles = list(range(7))
    xnT_p = a16(R0, [32, 896])

    def with_base(fn, base, *a, **k):
        nonlocal R1
        old = R1
        R1 = base
        try:
            fn(*a, **k)
        finally:
            R1 = old

    def prog():
        nonlocal xnT
        if not phase_ok():
            return
        norm_phase(pre_tiles, xnT_p, gmixT, "gmixT", wbase=R2)
        P.barrier()
        xnT_save = xnT
        xnT = xnT_p
        if phase_ok():
            for h in range(8):
                with_base(ret_unit, 60 * KW, h, pre_tiles, False, True, False, 0)
            P.barrier()
        if phase_ok():
            for un in range(8):
                with_base(hg_unit, 60 * KW, un, pre_tiles, False, True, False, 0)
            P.barrier()
        xnT = xnT_save
        for grp in range(2):
            tiles = list(range(7, 12)) if grp == 0 else list(range(12, 17))
            tok_off = grp * 640
            if not phase_ok():
                return
            norm_phase(tiles, xnT, gmixT, "gmixT")
            P.barrier()
            if not phase_ok():
                return
            for h in range(8):
                ret_unit(h, tiles, True, False, grp == 1, tok_off)
            P.barrier()
            if not phase_ok():
                return
            for un in range(8):
                hg_unit(un, tiles, True, False, grp == 1, tok_off)
            P.barrier()
            if not phase_ok():
                return
            merge_phase(tok_off)
            P.barrier()
            if not phase_ok():
                return
            wout_phase(tiles)
            P.barrier()
            if not phase_ok():
                return
            norm_phase([0, 1, 2, 3, 4], xn2T, gffnT, "gffnT", src_x1=True, wbase=R4 - 8192, dtok="actT2")
            P.barrier()
            if not phase_ok():
                return
            ffn_phase(grp)
            P.barrier()

    prog()
    P.barrier()

    with nc.Block() as block:
        P.emit(nc, block, esem, dsem)
    es.close()
    return nc


_NC_CACHE = {}


def _prep(inputs):
    f = lambda k: np.ascontiguousarray(np.asarray(inputs[k], dtype=np.float32))
    xp = f("x_prompt")
    xs = f("x_sample")
    consts = _consts()
    shared = {
        "w_in": f("w_in")[0], "w_br_ret": f("w_br_ret")[0], "w_br_hg": f("w_br_hg")[0], "w_out": f("w_out")[0],
        "w_gate": f("w_gate")[0], "w_up": f("w_up")[0], "w_down": f("w_down")[0],
        "g_mix": np.ascontiguousarray(f("norm_mix_g")[0].reshape(32, 128).T),
        "g_ffn": np.ascontiguousarray(f("norm_ffn_g")[0].reshape(32, 128).T),
        "g_fin": f("final_norm_g").reshape(1, D),
        "ret_g": f("ret_norm_g")[0].reshape(1, 2048), "hg_g": f("hg_norm_g")[0].reshape(1, 2048),
        "lbl": f("hg_lb_logits"),
        "k_dm": consts["dm"], "k_dm4": consts["dm4"], "k_dec": consts["dec"],
        "k_tri16": consts["tri16"], "k_blk16": consts["blk16"], "k_tri4": consts["tri4"], "k_blk4": consts["blk4"],
        "k_cm16": consts["cm16"], "k_cm4": consts["cm4"], "k_cmT": consts["cmT"], "k_cmT64": consts["cmT64"], "k_ident": consts["ident"],
    }
    cw = f("conv_w")[0]
    cb = f("conv_b")[0]
    cwb = np.concatenate([cw, cb[None, :]], axis=0)
    shared["cwb"] = np.ascontiguousarray(cwb.reshape(4, 86, 128).transpose(2, 1, 0).reshape(128, 86 * 4))
    sret = f("state_ret")[0]
    shg = f("state_hgrn")[0]
    sconv = f("state_ffn_conv")[0]
    in_maps = []
    for c in range(8):
        b, half = c // 2, c % 2
        xa = np.zeros((NT_ALL * 128, D), np.float32)
        pos = np.zeros((NT_ALL * 128,), np.float32)
        if half == 1:
            xa[0:1024] = xp[b, 0:1024]
            pos[0:1024] = np.arange(1024)
            xa[1024:2048] = xp[b, 1024:2048]
            pos[1024:2048] = np.arange(1024, 2048)
        else:
            xa[1024:2048] = xp[b, 0:1024]
            pos[1024:2048] = np.arange(1024)
        xa[2048:2112] = xs[16 * c:16 * c + 16].reshape(64, D)
        pos[2048:2112] = PAST + (np.arange(64) % 4)
        cs, sn = _rope_tables(pos)
        m = dict(shared)
        m["xa"] = xa
        m["cosT"] = np.ascontiguousarray(cs.reshape(NT_ALL, 128, 128))
        m["sinT"] = np.ascontiguousarray(sn.reshape(NT_ALL, 128, 128))
        m["sret"] = np.ascontiguousarray(sret[16 * c:16 * c + 16])
        m["shg"] = np.ascontiguousarray(shg[16 * c:16 * c + 16])
        m["sconv"] = np.ascontiguousarray(sconv[16 * c:16 * c + 16].reshape(32, FF))
        in_maps.append(m)
    return in_maps


def kernel(**inputs):
    in_maps = _prep(inputs)
    if "nc" not in _NC_CACHE:
        _NC_CACHE["nc"] = build_program()
    nc = _NC_CACHE["nc"]
    res = run_bass_kernel_spmd(nc, in_maps, core_ids=list(range(8)))
    return _assemble(res.results)


def _assemble(R):
    y_prompt = np.zeros((4, 2048, D), np.float32)
    y_sample = np.zeros((128, 4, D), np.float32)
    ret_p = np.zeros((1, 4, 8, 256, 256), np.float32)
    hg_p = np.zeros((1, 4, 16, 128, 128), np.float32)
    conv_p = np.zeros((1, 4, 2, FF), np.float32)
    ret_s = np.zeros((1, 128, 8, 256, 256), np.float32)
    hg_s = np.zeros((1, 128, 16, 128, 128), np.float32)
    conv_s = np.zeros((1, 128, 2, FF), np.float32)
    for c in range(8):
        b, half = c // 2, c % 2
        r = R[c]
        y_prompt[b, half * 1024:(half + 1) * 1024] = r["y_o"][0:1024]
        y_sample[16 * c:16 * c + 16] = r["y_o"][1024:1088].reshape(16, 4, D)
        ret_s[0, 16 * c:16 * c + 16] = r["rets_o"]
        hg_s[0, 16 * c:16 * c + 16] = r["hgs_o"]
        conv_s[0, 16 * c:16 * c + 16] = r["convs_o"].reshape(16, 2, FF)
        if half == 1:
            ret_p[0, b] = r["retp_o"]
            hg_p[0, b] = r["hgp_o"]
            conv_p[0, b] = r["convp_o"]
    return (y_prompt, y_sample, ret_p, hg_p, conv_p, ret_s, hg_s, conv_s)
```

```python
import numpy as np
import concourse.bass as bass
import concourse.mybir as mybir
from concourse.bass_utils import run_bass_kernel_spmd

F32 = mybir.dt.float32
BF16 = mybir.dt.bfloat16
ALU = mybir.AluOpType
AF = mybir.ActivationFunctionType

D = 4096
FF = 11008
NT_ALL = 17
EPS = 1e-6
PAST = 16384
C_QR, C_KR, C_VR, C_GR, C_FH, C_QH, C_IH, C_GH, C_GATE_R, C_GATE_H = (
    0, 2048, 4096, 6144, 8192, 10240, 12288, 14336, 16384, 20480)
GAM = [1.0 - 2.0 ** (-5.0 - h) for h in range(8)]


class Op:
    __slots__ = ("eng", "fn", "deps", "sig", "sigidx", "is_dma", "sem", "semval", "slot", "redirect")

    def __init__(self, eng, fn):
        self.eng = eng
        self.fn = fn
        self.deps = []
        self.sig = False
        self.sigidx = 0
        self.is_dma = False
        self.sem = None
        self.semval = 0
        self.slot = None
        self.redirect = None


class Prog:
    ENGS = ("pe", "act", "dve", "pool", "sp")

    def __init__(self):
        self.streams = {e: [] for e in self.ENGS}
        self.lastw = {}
        self.readers = {}
        self.slot_cnt = {}
        self.slot_last = {}
        self.rr = {}

    def _add(self, op, reads, writes):
        deps = {}
        for t in reads:
            w = self.lastw.get(t)
            if w is not None:
                deps[id(w)] = w
            if t.startswith("ps"):
                for r in self.readers.get(t, {}).values():
                    if r.eng != op.eng:
                        deps[id(r)] = r
        for t in writes:
            w = self.lastw.get(t)
            if w is not None:
                deps[id(w)] = w
            for r in self.readers.get(t, {}).values():
                deps[id(r)] = r
        for t in reads:
            rd = self.readers.setdefault(t, {})
            key = op.eng if not op.is_dma else ("dma", op.slot)
            rd[key] = op
        for t in writes:
            self.lastw[t] = op
            self.readers[t] = {}
        deps.pop(id(op), None)
        op.deps = list(deps.values())
        self.streams[op.eng].append(op)
        return op

    def op(self, eng, fn, reads=(), writes=()):
        return self._add(Op(eng, fn), reads, writes)

    SLOT_RR = {"c": 4, "x": 2, "st": 3, "so": 3, "og": 2, "y": 2, "cv": 2, "m": 1, "w0": 1, "w1": 1, "w2": 1}

    def dma(self, eng, slot, fn, reads=(), writes=()):
        n = self.SLOT_RR[slot]
        k = self.rr.get(slot, 0)
        self.rr[slot] = k + 1
        slot = "%s_%d" % (slot, k % n)
        o = Op(eng, fn)
        o.is_dma = True
        o.slot = slot
        self.slot_cnt[slot] = self.slot_cnt.get(slot, 0) + 16
        o.semval = self.slot_cnt[slot]
        prev = self.slot_last.get(slot)
        self.slot_last[slot] = o
        self._add(o, reads, writes)
        if prev is not None and all(d is not prev for d in o.deps):
            o.deps.append(prev)
        return o

    @classmethod
    def all_slots(cls):
        return ["%s_%d" % (s, i) for s, n in cls.SLOT_RR.items() for i in range(n)]

    def barrier(self):
        lasts = []
        for e in self.ENGS:
            for o in reversed(self.streams[e]):
                if not o.is_dma and o.fn is not None:
                    lasts.append(o)
                    break
        dmas = list(self.slot_last.values())
        for e in self.ENGS:
            b = Op(e, None)
            for l in lasts:
                if l.eng != e:
                    b.deps.append(l)
            b.deps.extend(dmas)
            self.streams[e].append(b)
        self.lastw = {}
        self.readers = {}

    def emit(self, nc, block, esem, dsem):
        for e in self.ENGS:
            for o in self.streams[e]:
                nd = []
                for d in o.deps:
                    if not d.is_dma and d.redirect is not None:
                        d = d.redirect
                    if d is o:
                        continue
                    nd.append(d)
                    if d.is_dma:
                        continue
                    if d.eng == o.eng and o.eng == "pe" and not o.is_dma:
                        continue
                    d.sig = True
                o.deps = nd
        for e in self.ENGS:
            c = 0
            for o in self.streams[e]:
                if o.sig and not o.is_dma and o.fn is not None:
                    c += 1
                    o.sigidx = c
        handles = {"pe": "tensor", "act": "scalar", "dve": "vector", "pool": "gpsimd", "sp": "sync"}

        def run(e, eng):
            waited = {}
            for o in self.streams[e]:
                for d in o.deps:
                    if d.is_dma:
                        key, val, sem = ("d", d.slot), d.semval, dsem[d.slot]
                    else:
                        if d.eng == e and e == "pe":
                            continue
                        key, val, sem = ("e", d.eng), d.sigidx, esem[d.eng]
                    if waited.get(key, 0) >= val:
                        continue
                    waited[key] = val
                    eng.wait_ge(sem, val)
                if o.fn is None:
                    continue
                ins = o.fn(eng)
                if o.is_dma:
                    ins.then_inc(dsem[o.slot], 16)
                elif o.sig:
                    ins.then_inc(esem[e], 1)

        @block.tensor
        def _(eng):
            run("pe", eng)

        @block.scalar
        def _(eng):
            run("act", eng)

        @block.vector
        def _(eng):
            run("dve", eng)

        @block.gpsimd
        def _(eng):
            run("pool", eng)

        @block.sync
        def _(eng):
            run("sp", eng)


def _consts():
    c = {}
    idx = np.arange(128)
    diff = idx[None, :] - idx[:, None]
    dm = np.zeros((128, 8, 128), np.float32)
    dm4 = np.zeros((128, 8, 128), np.float32)
    qd = np.zeros((128, 8), np.float32)
    kd = np.zeros((128, 8), np.float32)
    qd4 = np.zeros((128, 8), np.float32)
    kd4 = np.zeros((128, 8), np.float32)
    same4 = (idx[None, :] // 4) == (idx[:, None] // 4)
    for h in range(8):
        lg = np.log1p(-np.exp2(np.float32(-5.0 - h))).astype(np.float32)
        dm[:, h, :] = np.where(diff >= 0, np.exp(np.where(diff >= 0, diff, 0).astype(np.float32) * lg), 0.0)
        dm4[:, h, :] = np.where((diff >= 0) & same4, np.exp(np.where(diff >= 0, diff, 0).astype(np.float32) * lg), 0.0)
        qd[:, h] = np.exp((idx + 1.0).astype(np.float32) * lg)
        kd[:, h] = np.exp((127.0 - idx).astype(np.float32) * lg)
        qd4[:, h] = np.exp(((idx % 4) + 1.0).astype(np.float32) * lg)
        kd4[:, h] = np.exp((3.0 - (idx % 4)).astype(np.float32) * lg)
    c["dm"] = dm
    c["dm4"] = dm4
    c["dec"] = np.concatenate([qd, kd, qd4, kd4], axis=1).astype(np.float32)
    same16 = (idx[None, :] // 16) == (idx[:, None] // 16)
    c["tri16"] = ((diff >= 0) & same16).astype(np.float32)
    c["blk16"] = same16.astype(np.float32)
    c["tri4"] = ((diff >= 0) & same4).astype(np.float32)
    c["blk4"] = same4.astype(np.float32)
    cm16 = np.zeros((8, 128), np.float32)
    for ch in range(8):
        cm16[ch, ch * 16:(ch + 1) * 16] = 1.0
    cm4 = np.zeros((16, 128), np.float32)
    for s in range(16):
        cm4[s, s * 4:(s + 1) * 4] = 1.0
    c["cm16"] = np.broadcast_to(cm16.reshape(1, 8 * 128), (128, 1024)).copy()
    c["cm4"] = np.broadcast_to(cm4.reshape(1, 16 * 128), (128, 2048)).copy()
    c["cmT"] = np.concatenate([cm16.T, cm4.T], axis=1).astype(np.float32).copy()
    cmT64 = np.zeros((128, 64), np.float32)
    cmT64[:, 0:8] = cm16.T
    cmT64[:, 32:48] = cm4.T
    c["cmT64"] = cmT64
    c["ident"] = np.eye(128, dtype=np.float32)
    return c


def _rope_tables(pos):
    half = 128
    inv = (np.float32(10000.0) ** (-np.arange(half, dtype=np.float32) / np.float32(half))).astype(np.float32)
    ang = (pos.astype(np.float32)[:, None] * inv[None, :]).astype(np.float32)
    return np.cos(ang).astype(np.float32), np.sin(ang).astype(np.float32)


def build_program():
    nc = bass.Bass("TRN2", target_bir_lowering=False)

    def din(name, shape, dt=F32):
        return nc.dram_tensor(name, list(shape), dt, kind="ExternalInput").ap()

    def dout(name, shape, dt=F32):
        return nc.dram_tensor(name, list(shape), dt, kind="ExternalOutput").ap()

    xa = din("xa", [NT_ALL * 128, D])
    w_in = din("w_in", [D, 24576])
    w_br_ret = din("w_br_ret", [2048, D])
    w_br_hg = din("w_br_hg", [2048, D])
    w_out = din("w_out", [D, D])
    w_gate = din("w_gate", [D, FF])
    w_up = din("w_up", [D, FF])
    w_down = din("w_down", [FF, D])
    g_mix = din("g_mix", [128, 32])
    g_ffn = din("g_ffn", [128, 32])
    cwb_d = din("cwb", [128, 86 * 4])
    g_fin = din("g_fin", [1, D])
    ret_g = din("ret_g", [1, 2048])
    hg_g = din("hg_g", [1, 2048])
    lbl = din("lbl", [2, 2048])
    sret = din("sret", [16, 8, 256, 256])
    shg = din("shg", [16, 16, 128, 128])
    sconv = din("sconv", [32, FF])
    cosT = din("cosT", [NT_ALL, 128, 128])
    sinT = din("sinT", [NT_ALL, 128, 128])
    k_dm = din("k_dm", [128, 8, 128])
    k_dm4 = din("k_dm4", [128, 8, 128])
    k_dec = din("k_dec", [128, 32])
    k_tri16 = din("k_tri16", [128, 128])
    k_blk16 = din("k_blk16", [128, 128])
    k_tri4 = din("k_tri4", [128, 128])
    k_blk4 = din("k_blk4", [128, 128])
    k_cm16 = din("k_cm16", [128, 1024])
    k_cm4 = din("k_cm4", [128, 2048])
    k_cmT = din("k_cmT", [128, 24])
    k_cmT64 = din("k_cmT64", [128, 64])
    k_ident = din("k_ident", [128, 128])

    y_o = dout("y_o", [1088, D])
    retp_o = dout("retp_o", [8, 256, 256])
    hgp_o = dout("hgp_o", [16, 128, 128])
    convp_o = dout("convp_o", [2, FF])
    rets_o = dout("rets_o", [16, 8, 256, 256])
    hgs_o = dout("hgs_o", [16, 16, 128, 128])
    convs_o = dout("convs_o", [32, FF])

    og_scr = nc.dram_tensor("og_scr", [32, 128, 1280], BF16, kind="Internal").ap()
    sr_scr = nc.dram_tensor("sr_scr", [8, 256, 256], F32, kind="Internal").ap()
    sh_scr = nc.dram_tensor("sh_scr", [16, 128, 128], F32, kind="Internal").ap()

    P = Prog()
    from contextlib import ExitStack
    es = ExitStack()

    def sb(name, shape, dt=F32):
        return es.enter_context(nc.sbuf_tensor(name, list(shape), dt))

    AW = 46 * 1024
    arena = sb("arena", [128, AW])
    ident_f = sb("ident_f", [128, 128])
    ident_b = sb("ident_b", [128, 128], BF16)
    dm = sb("dm", [128, 8, 128])
    dm4 = sb("dm4", [128, 8, 128])
    dec = sb("dec", [128, 32])
    tri16 = sb("tri16", [128, 128])
    blk16 = sb("blk16", [128, 128])
    tri4 = sb("tri4", [128, 128])
    blk4 = sb("blk4", [128, 128])
    cm16 = sb("cm16", [128, 8, 128], BF16)
    cm4 = sb("cm4", [128, 16, 128], BF16)
    cmT = sb("cmT", [128, 24])
    tri16b = sb("tri16b", [128, 128], BF16)
    blk16b = sb("blk16b", [128, 128], BF16)
    tri4b = sb("tri4b", [128, 128], BF16)
    blk4b = sb("blk4b", [128, 128], BF16)
    cmTb = sb("cmTb", [128, 64], BF16)
    gmixT = sb("gmixT", [128, 32])
    gffnT = sb("gffnT", [128, 32])
    small = sb("small", [128, 64])
    carry = sb("carry", [128, 86, 2])
    cwb = sb("cwb_s", [128, 86, 4])

    psum = [es.enter_context(nc.psum_tensor("ps%d" % i, [128, 512], F32)) for i in range(6)]
    pskv2 = [es.enter_context(nc.psum_tensor("pskv%d" % i, [128, 512], F32)) for i in range(2)]

    def kvview(c):
        return pskv2[c // 4][:, (c % 4) * 128:(c % 4 + 1) * 128]

    def a32(off, shape):
        n = int(np.prod(shape))
        v = arena[:, off:off + n]
        if len(shape) == 2:
            return v.rearrange("p (a b) -> p a b", a=shape[0])
        if len(shape) == 3:
            return v.rearrange("p (a b c) -> p a b c", a=shape[0], b=shape[1])
        return v

    def a16(off, shape):
        n = int(np.prod(shape))
        assert n % 2 == 0
        v = arena[:, off:off + n // 2].bitcast(BF16)
        if len(shape) == 2:
            return v.rearrange("p (a b) -> p a b", a=shape[0])
        if len(shape) == 3:
            return v.rearrange("p (a b c) -> p a b c", a=shape[0], b=shape[1])
        return v

    KW = 1024 // 4
    R0, R1, R2, R3, R4 = 0, 40 * KW, 80 * KW, 120 * KW, 168 * KW
    xnT = a16(R0, [32, 640])
    ogT = a16(R1, [32, 640])
    mgT = a16(R2, [32, 640])
    xn2T = mgT
    x1 = a32(R0, [5, 4096])
    wring = [a16(R3 + i * 16 * KW, [8192]) for i in range(3)]
    wstate = {"i": 0}

    sem_names = ["w0", "w1", "w2", "x", "c", "st", "so", "og", "y", "cv", "m"]
    esem = {e: es.enter_context(nc.semaphore("se_" + e)) for e in ("pe", "act", "dve", "pool")}
    dsem = {s: es.enter_context(nc.semaphore("sd_" + s)) for s in Prog.all_slots()}

    uid = [0]

    def tok(prefix):
        uid[0] += 1
        return "%s#%d" % (prefix, uid[0])

    open_groups = {}

    def mm(out, lhsT, rhs, start, stop, reads, writes):
        o = P.op("pe", lambda e: e.matmul(out, lhsT, rhs, start=start, stop=stop), reads, writes)
        key = writes[0]
        if start:
            assert key not in open_groups, ("accumulation group still open on", key)
            open_groups[key] = []
        open_groups[key].append(o)
        if stop:
            for g in open_groups.pop(key):
                if g is not o:
                    g.redirect = o

    def tr(out, in_, idn, reads, writes):
        P.op("pe", lambda e: e.transpose(out, in_, idn), reads, writes)

    def act(out, in_, func, reads, writes, scale=1.0, bias=0.0, accum=None):
        if accum is None:
            P.op("act", lambda e: e.activation(out, in_, func, bias=bias, scale=scale), reads, writes)
        else:
            P.op("act", lambda e: e.activation(out, in_, func, bias=bias, scale=scale, accum_out=accum),
                 reads, writes)

    def tt(eng, out, in0, in1, op, reads, writes):
        eng = "dve" if eng == "pool" else eng
        P.op(eng, lambda e: e.tensor_tensor(out, in0, in1, op), reads, writes)

    def ts(eng, out, in0, s1, s2, op0, op1, reads, writes):
        eng = "dve" if eng == "pool" else eng
        if s2 is None:
            P.op(eng, lambda e: e.tensor_scalar(out, in0, s1, None, op0), reads, writes)
        else:
            P.op(eng, lambda e: e.tensor_scalar(out, in0, s1, s2, op0, op1), reads, writes)

    def stt(eng, out, in0, sc, in1, op0, op1, reads, writes):
        P.op(eng, lambda e: e.scalar_tensor_tensor(out, in0, sc, in1, op0, op1), reads, writes)

    def cp(eng, out, in_, reads, writes):
        eng = "act" if eng == "pool" else eng
        if eng == "act":
            P.op("act", lambda e: e.copy(out, in_), reads, writes)
        else:
            P.op(eng, lambda e: e.tensor_copy(out, in_), reads, writes)

    def dma(slot, out, in_, reads, writes, eng="sp"):
        P.dma(eng, slot, lambda e: e.dma_start(out=out, in_=in_), reads, writes)

    def load_w(src, kcn, cols):
        i = wstate["i"] % 3
        wstate["i"] += 1
        buf = wring[i][:, 0:kcn * cols].rearrange("p (k c) -> p k c", k=kcn)
        t = "wring%d" % i
        P.dma("pool", "w%d" % i,
              lambda e: e.dma_start(out=buf, in_=src.rearrange("(k p) c -> p k c", p=128)),
              (), (t,))
        return buf, t

    def rstd_from_ss(ss, n, reads_writes_tok):
        act(ss, ss, AF.Sqrt, (reads_writes_tok,), (reads_writes_tok,), scale=1.0 / n, bias=EPS)
        P.op("dve", lambda e: e.reciprocal(ss, ss), (reads_writes_tok,), (reads_writes_tok,))

    def load_consts():
        for dst, src, name in ((ident_f, k_ident, "ident_f"), (dm, k_dm, "dm"), (dm4, k_dm4, "dm4"),
                               (dec, k_dec, "dec"), (tri16, k_tri16, "tri16"), (blk16, k_blk16, "blk16"),
                               (tri4, k_tri4, "tri4"), (blk4, k_blk4, "blk4"), (cmT, k_cmT, "cmT")):
            dma("c", dst[:], src, (), (name,))
        dma("c", gmixT[:], g_mix, (), ("gmixT",))
        dma("c", gffnT[:], g_ffn, (), ("gffnT",))
        dma("c", cwb[:], cwb_d.rearrange("p (a b) -> p a b", b=4), (), ("cw",))
        P.dma("pool", "m", lambda e: e.dma_start(out=ident_b[:], in_=k_ident), (), ("ident_b",))
        for dst_, src_, nm_ in ((tri16b, k_tri16, "tri16b"), (blk16b, k_blk16, "blk16b"), (tri4b, k_tri4, "tri4b"),
                                (blk4b, k_blk4, "blk4b"), (cmTb, k_cmT64, "cmTb")):
            P.dma("pool", "m", lambda e, dst_=dst_, src_=src_: e.dma_start(out=dst_[:], in_=src_), (), (nm_,))
        P.dma("pool", "m", lambda e: e.dma_start(out=cm16[:], in_=k_cm16.rearrange("p (a b) -> p a b", a=8)),
              (), ("cm16",))
        P.dma("pool", "m", lambda e: e.dma_start(out=cm4[:], in_=k_cm4.rearrange("p (a b) -> p a b", a=16)),
              (), ("cm4",))
        P.op("dve", lambda e: e.memset(carry[:], 0.0), (), ("carry",))

    def norm_phase(tiles, dstT, gT, gtok, src_x1=False, wbase=R1, dtok="actT"):
        xt = [a32(wbase + i * 4096, [4096]) for i in range(2)]
        xb = [a16(wbase + 8192 + i * 2048, [4096]) for i in range(2)]
        for li, tt_ in enumerate(tiles):
            b = li % 2
            if src_x1:
                src = x1[:, tt_, :]
                srct = "x1_%d" % tt_
            else:
                src = xt[b]
                srct = "xt%d" % b
                dma("x", xt[b], xa[tt_ * 128:(tt_ + 1) * 128, :], (), (srct,))
            ss = small[:, 2 * b:2 * b + 1]
            sst = "nss%d" % b
            xbt = "xb%d" % b
            act(xb[b], src, AF.Square, (srct,), (xbt, sst), accum=ss)
            rstd_from_ss(ss, D, sst)
            ts("dve", xb[b], src, ss, None, ALU.mult, None, (srct, sst), (xbt,))
            for k4 in range(8):
                pt = psum[2]
                ptk = "ps2"
                pv = pt[:, (k4 % 2) * 256:(k4 % 2) * 256 + 256].bitcast(BF16).rearrange("p (a b) -> p a b", a=4)
                for q in range(4):
                    kc = k4 * 4 + q
                    tr(pv[:, q, :], xb[b][:, kc * 128:(kc + 1) * 128], ident_b[:], (xbt, "ident_b"), (ptk,))
                for q in range(4):
                    kc = k4 * 4 + q
                    eng = "act" if q % 2 == 0 else "dve"
                    o = dstT[:, kc, li * 128:(li + 1) * 128]
                    if eng == "act":
                        act(o, pv[:, q, :], AF.Copy, (ptk, gtok), (dtok,), scale=gT[:, kc:kc + 1])
                    else:
                        ts("dve", o, pv[:, q, :], gT[:, kc:kc + 1], None, ALU.mult, None, (ptk, gtok), (dtok,))

    def proj(srcT, srct, ntile, wsrc, ncols, consume, kcn=32, koff=0):
        wb, wt = load_w(wsrc, kcn, ncols)
        for ti in range(ntile):
            pb = ti % 2
            ps = psum[pb][:, 0:ncols]
            for kc in range(kcn):
                mm(ps, srcT[:, koff + kc, ti * 128:(ti + 1) * 128], wb[:, kc, :], kc == 0, kc == kcn - 1,
                   (srct, wt), ("ps%d" % pb,))
            consume(ti, ps, "ps%d" % pb)

    def ret_unit(h, tiles, outputs, first, last_store_out, tok_off):
        nt = len(tiles)
        W = R1
        st_q = a32(W, [nt, 256]); W += nt * 256
        st_k = a32(W, [nt, 256]); W += nt * 256
        sg = a32(W, [nt, 256]); W += nt * 256
        st_v = a16(W, [nt, 256]); W += nt * 128
        tmp = [a32(W + i * 128, [128]) for i in range(4)]; W += 512
        q_bf = a16(W, [256]); W += 128
        k_bf = a16(W, [256]); W += 128
        qd_bf = a16(W, [256]); W += 128
        kd_bf = a16(W, [256]); W += 128
        qkT = a16(W, [6, 128]); W += 384
        scT = a16(W, [128]); W += 64
        S = a32(W, [2, 256]); W += 512
        S_bf = a16(W, [2, 256]); W += 256
        og_bf = a16(W, [256]); W += 128
        ogTt = a16(W, [2, 128]); W += 128
        gsl = a32(W, [256]); W += 256
        cs = a32(W, [nt, 128]); W += nt * 128
        sn = a32(W, [nt, 128]); W += nt * 128
        kdm = a16(W, [256]); W += 128
        if 16 in tiles:
            S0 = a32(W, [2, 8, 256]); W += 4096
            S0b = a16(W, [2, 8, 256]); W += 2048
            Sn = a32(W, [2, 8, 256]); W += 4096
            qdm = a16(W, [2, 8, 128]); W += 1024
        osum = a32(W, [256]); W += 256
        assert W <= R3, W
        u = "r_"

        for li, tg in enumerate(tiles):
            dma("c", cs[:, li, :], cosT[tg], (), (u + "cs",))
            dma("c", sn[:, li, :], sinT[tg], (), (u + "sn",))
        if outputs:
            dma("c", gsl, ret_g[0:1, h * 256:(h + 1) * 256].to_broadcast([128, 256]), (), (u + "gsl",))

        def ev_q(ti, ps, pt):
            cp("act", st_q[:, ti, :], ps, (pt,), (u + "stq%d" % ti,))

        def ev_k(ti, ps, pt):
            act(st_k[:, ti, :], ps, AF.Copy, (pt,), (u + "stk%d" % ti,), scale=1.0 / 16.0)

        def ev_v(ti, ps, pt):
            cp("act", st_v[:, ti, :], ps, (pt,), (u + "stv%d" % ti,))

        def ev_g(ti, ps, pt):
            act(sg[:, ti, :], ps, AF.Silu, (pt,), (u + "sg%d" % ti,))
            tt("pool", sg[:, ti, :], sg[:, ti, :], gsl, ALU.mult, (u + "sg%d" % ti, u + "gsl"), (u + "sg%d" % ti,))

        if outputs:
            proj(xnT, "actT", nt, w_in[:, C_QR + h * 256:C_QR + (h + 1) * 256], 256, ev_q)
        proj(xnT, "actT", nt, w_in[:, C_KR + h * 256:C_KR + (h + 1) * 256], 256, ev_k)
        proj(xnT, "actT", nt, w_in[:, C_VR + h * 256:C_VR + (h + 1) * 256], 256, ev_v)
        if outputs:
            proj(xnT, "actT", nt, w_in[:, C_GR + h * 256:C_GR + (h + 1) * 256], 256, ev_g)

        St = u + "S"
        if first:
            P.op("dve", lambda e: e.memset(S, 0.0), (), (St,))
        else:
            dma("st", S, sr_scr[h].rearrange("(c p) v -> p c v", p=128), ("sr_scr%d" % h,), (St,))
        if outputs:
            cp("pool", S_bf, S, (St,), (u + "Sbf",))

        def rope(st, ti, dst, scale, name):
            x1_, x2_ = st[:, ti, 0:128], st[:, ti, 128:256]
            c_, s_ = cs[:, ti, :], sn[:, ti, :]
            rd = (name + "%d" % ti, u + "cs", u + "sn")
            tt("dve", tmp[0], x1_, c_, ALU.mult, rd, (u + "t0",))
            tt("pool", tmp[1], x2_, s_, ALU.mult, rd, (u + "t1",))
            tt("pool", tmp[2], x1_, s_, ALU.mult, rd, (u + "t2",))
            tt("dve", tmp[3], x2_, c_, ALU.mult, rd, (u + "t3",))
            tt("dve", dst[:, 0:128], tmp[0], tmp[1], ALU.subtract, (u + "t0", u + "t1"), (u + dst_name[id(dst)],))
            tt("pool", dst[:, 128:256], tmp[2], tmp[3], ALU.add, (u + "t2", u + "t3"), (u + dst_name[id(dst)],))

        dst_name = {id(q_bf): "qbf", id(k_bf): "kbf"}

        for ti, tg in enumerate(tiles):
            sample = (tg == 16)
            mask = dm4 if sample else dm
            mtok = "dm4" if sample else "dm"
            qcol = 16 if sample else 0
            kcol = 24 if sample else 8
            rope(st_k, ti, k_bf, 1.0 / 16.0, u + "stk")
            ts("dve", kd_bf, k_bf, dec[:, kcol + h:kcol + h + 1], None, ALU.mult, None, (u + "kbf", "dec"), (u + "kdbf",))
            if outputs:
                rope(st_q, ti, q_bf, 1.0, u + "stq")
                ts("pool", qd_bf, q_bf, dec[:, qcol + h:qcol + h + 1], None, ALU.mult, None, (u + "qbf", "dec"),
                   (u + "qdbf",))
                pv = psum[2][:, 0:384].bitcast(BF16).rearrange("p (a b) -> p a b", a=6)
                for c in range(2):
                    tr(pv[:, c, :], q_bf[:, c * 128:(c + 1) * 128], ident_b[:], (u + "qbf", "ident_b"), ("ps2",))
                    tr(pv[:, 2 + c, :], k_bf[:, c * 128:(c + 1) * 128], ident_b[:], (u + "kbf", "ident_b"), ("ps2",))
                    tr(pv[:, 4 + c, :], qd_bf[:, c * 128:(c + 1) * 128], ident_b[:], (u + "qdbf", "ident_b"), ("ps2",))
                cp("act", qkT, pv, ("ps2",), (u + "qkT",))
                sc = psum[3][:, 0:128]
                for c in range(2):
                    mm(sc, qkT[:, 2 + c, :], qkT[:, c, :], c == 0, c == 1, (u + "qkT",), ("ps3",))
                tt("dve", scT, sc, mask[:, h, :], ALU.mult, ("ps3", mtok), (u + "scT",))
                o = psum[4][:, 0:256]
                vtk = u + "stv%d" % ti
                if not sample:
                    mm(o, scT, st_v[:, ti, :], True, False, (u + "scT", vtk), ("ps4",))
                    for c in range(2):
                        mm(o, qkT[:, 4 + c, :], S_bf[:, c, :], False, c == 1, (u + "qkT", u + "Sbf"), ("ps4",))
            if not sample:
                kv = psum[5][:, 0:512].rearrange("p (c v) -> p c v", c=2)
                for c in range(2):
                    mm(kv[:, c, :], kd_bf[:, c * 128:(c + 1) * 128], st_v[:, ti, :], True, True,
                       (u + "kdbf", u + "stv%d" % ti), ("ps5",))
                sdec = float(np.float32(GAM[h]) ** 128)
                stt("dve", S, S, sdec, kv, ALU.mult, ALU.add, (St, "ps5"), (St,))
                if outputs:
                    cp("pool", S_bf, S, (St,), (u + "Sbf",))
            else:
                sdec = float(np.float32(GAM[h]) ** 4)
                for half in range(2):
                    s0t = u + "S0"
                    for c_ in range(2):
                        dma("st", S0[:, c_], sret[half * 8:(half + 1) * 8, h, c_ * 128:(c_ + 1) * 128, :]
                            .rearrange("s p v -> p s v"), (), (s0t,))
                    cp("pool", S0b, S0, (s0t,), (u + "S0b",))
                    for c_ in range(2):
                        P.op("dve", lambda e, c_=c_, half=half: e.tensor_tensor(
                            qdm[:, c_], qkT[:, 4 + c_, :].unsqueeze(1).to_broadcast([128, 8, 128]),
                            cm4[:, half * 8:(half + 1) * 8, :], ALU.mult),
                            (u + "qkT", "cm4"), (u + "qdm",))
                    oh_ = psum[4][:, half * 256:(half + 1) * 256]
                    if half == 0:
                        mm(oh_, scT, st_v[:, ti, :], True, False, (u + "scT", u + "stv%d" % ti), ("ps4",))
                    for s in range(8):
                        for c in range(2):
                            mm(oh_, qdm[:, c, s, :], S0b[:, c, s, :], (half == 1 and s == 0 and c == 0),
                               (s == 7 and c == 1), (u + "qdm", u + "S0b"), ("ps4",))
                    for s in range(8):
                        sg_ = half * 8 + s
                        ts("pool", kdm, kd_bf, cmT[:, 8 + sg_:9 + sg_], None, ALU.mult, None, (u + "kdbf", "cmT"),
                           (u + "kdm",))
                        kv = psum[5][:, 0:512].rearrange("p (c v) -> p c v", c=2)
                        for c in range(2):
                            mm(kv[:, c, :], kdm[:, c * 128:(c + 1) * 128], st_v[:, ti, :], True, True,
                               (u + "kdm", u + "stv%d" % ti), ("ps5",))
                        stt("dve", Sn[:, :, s, :], S0[:, :, s, :], sdec, kv, ALU.mult, ALU.add, (s0t, "ps5"),
                            (u + "Sn",))
                    for c_ in range(2):
                        dma("so", rets_o[half * 8:(half + 1) * 8, h, c_ * 128:(c_ + 1) * 128, :]
                            .rearrange("s p v -> p s v"), Sn[:, c_], (u + "Sn",), ())
            if outputs:
                ss = small[:, 8:9]
                cp("act", osum, psum[4][:, 0:256], ("ps4",), (u + "osum",))
                if sample:
                    tt("dve", osum, psum[4][:, 256:512], osum, ALU.add, ("ps4", u + "osum"), (u + "osum",))
                osrc, osrct = osum, u + "osum"
                act(og_bf, osrc, AF.Square, (osrct,), (u + "ogbf", u + "ss"), accum=ss)
                rstd_from_ss(ss, 256, u + "ss")
                stt("dve", og_bf, osrc, ss, sg[:, ti, :], ALU.mult, ALU.mult, (osrct, u + "ss", u + "sg%d" % ti),
                    (u + "ogbf",))
                pv2 = psum[2][:, 0:128].bitcast(BF16).rearrange("p (a b) -> p a b", a=2)
                for c in range(2):
                    tr(pv2[:, c, :], og_bf[:, c * 128:(c + 1) * 128], ident_b[:], (u + "ogbf", "ident_b"), ("ps2",))
                cp("act", ogTt, pv2, ("ps2",), (u + "ogT",))
                t0 = tok_off + ti * 128
                dma("og", og_scr[h * 2:h * 2 + 2, :, t0:t0 + 128].rearrange("c p t -> p c t"), ogTt,
                    (u + "ogT",), ("og_scr",))
        dma("st", sr_scr[h].rearrange("(c p) v -> p c v", p=128), S, (St,), ("sr_scr%d" % h,))
        if last_store_out:
            dma("so", retp_o[h].rearrange("(c p) v -> p c v", p=128), S, (St,), ())

    def hg_unit(un, tiles, outputs, first, last_store_out, tok_off):
        nt = len(tiles)
        W = R1
        sgf = a32(W, [nt, 256]); W += nt * 256
        lgf = a32(W, [nt, 256]); W += nt * 256
        kh = a32(W, [nt, 256]); W += nt * 256
        if outputs:
            st_q = a32(W, [nt, 256]); W += nt * 256
            sg = a32(W, [nt, 256]); W += nt * 256
        vh = a16(W, [nt, 256]); W += nt * 128
        lb = a32(W, [256]); W += 256
        oml = a32(W, [256]); W += 256
        l1 = a32(W, [256]); W += 256
        gsl = a32(W, [256]); W += 256
        b_sb = a32(W, [256]); W += 256
        eb = a32(W, [256]); W += 256
        enb = a32(W, [256]); W += 256
        lh = a16(W, [256]); W += 128
        ll = a16(W, [256]); W += 128
        dk = a32(W, [256]); W += 256
        qt_bf = a16(W, [256]); W += 128
        kt_bf = a16(W, [256]); W += 128
        kd_bf = a16(W, [256]); W += 128
        qkT = a16(W, [4, 128]); W += 256
        ebT = a32(W, [2, 32]); W += 64
        scT = a16(W, [128]); W += 64
        kdm = a16(W, [8, 128]); W += 512
        Sb = [a32(W + i * 1152, [9, 128]) for i in range(2)]; W += 2304
        Sbb = a16(W, [8, 128]); W += 512
        qTm = a16(W, [8, 128]); W += 512
        og_bf = a16(W, [256]); W += 128
        ogTt = a16(W, [2, 128]); W += 128
        if 16 in tiles:
            S0 = a32(W, [16, 128]); W += 2048
            S0b = a16(W, [16, 128]); W += 1024
            Sn = a32(W, [16, 128]); W += 2048
        osum = a32(W, [128]); W += 128
        assert W <= R3, W
        u = "h_"
        c0 = un * 256

        dma("c", lb, lbl[0:1, c0:c0 + 256].to_broadcast([128, 256]), (), (u + "lb",))
        dma("c", l1, lbl[1:2, c0:c0 + 256].to_broadcast([128, 256]), (), (u + "l1",))
        tt("dve", lb, lb, l1, ALU.subtract, (u + "lb", u + "l1"), (u + "lb",))
        act(lb, lb, AF.Sigmoid, (u + "lb",), (u + "lb",))
        ts("dve", oml, lb, -1.0, 1.0, ALU.mult, ALU.add, (u + "lb",), (u + "oml",))
        if outputs:
            dma("c", gsl, hg_g[0:1, c0:c0 + 256].to_broadcast([128, 256]), (), (u + "gsl",))

        def ev_f(ti, ps, pt):
            t = u + "f%d" % ti
            act(sgf[:, ti, :], ps, AF.Sigmoid, (pt,), (t,))
            tt("dve", sgf[:, ti, :], sgf[:, ti, :], oml, ALU.mult, (t, u + "oml"), (t,))
            tt("dve", sgf[:, ti, :], sgf[:, ti, :], lb, ALU.add, (t, u + "lb"), (t,))
            act(lgf[:, ti, :], sgf[:, ti, :], AF.Ln, (t,), (u + "lgf%d" % ti,))
            ts("pool", kh[:, ti, :], sgf[:, ti, :], -1.0, 1.0, ALU.mult, ALU.add, (t,), (u + "kh%d" % ti,))

        def ev_q(ti, ps, pt):
            cp("dve", st_q[:, ti, :], ps, (pt,), (u + "stq%d" % ti,))

        def ev_i(ti, ps, pt):
            act(vh[:, ti, :], ps, AF.Silu, (pt,), (u + "vh%d" % ti,))

        def ev_g(ti, ps, pt):
            act(sg[:, ti, :], ps, AF.Silu, (pt,), (u + "sg%d" % ti,))
            tt("pool", sg[:, ti, :], sg[:, ti, :], gsl, ALU.mult, (u + "sg%d" % ti, u + "gsl"), (u + "sg%d" % ti,))

        proj(xnT, "actT", nt, w_in[:, C_FH + c0:C_FH + c0 + 256], 256, ev_f)
        if outputs:
            proj(xnT, "actT", nt, w_in[:, C_QH + c0:C_QH + c0 + 256], 256, ev_q)
        proj(xnT, "actT", nt, w_in[:, C_IH + c0:C_IH + c0 + 256], 256, ev_i)
        if outputs:
            proj(xnT, "actT", nt, w_in[:, C_GH + c0:C_GH + c0 + 256], 256, ev_g)

        for hh in range(2):
            hd = un * 2 + hh
            if first:
                P.op("dve", lambda e, hh=hh: e.memset(Sb[hh][:, 0, :], 0.0), (), (u + "S%d" % hh,))
            else:
                dma("st", Sb[hh][:, 0, :], sh_scr[hd], ("sh_scr%d" % hd,), (u + "S%d" % hh,))

        import os
        hgdbg = int(os.environ.get("HGDBG", "0"))
        for ti, tg in enumerate(tiles):
            if hgdbg == 1:
                break
            sample = (tg == 16)
            tri, blk = (tri4, blk4) if sample else (tri16, blk16)
            trit, blkt = ("tri4", "blk4") if sample else ("tri16", "blk16")
            ncm = 16 if sample else 8
            step = 4 if sample else 16
            trib, blkb = (tri4b, blk4b) if sample else (tri16b, blk16b)
            tribt, blkbt = ("tri4b", "blk4b") if sample else ("tri16b", "blk16b")
            cs_ = psum[3][:, 0:512].rearrange("p (a b) -> p a b", a=2)
            lt = u + "lgf%d" % ti
            cp("pool", lh, lgf[:, ti, :], (lt,), (u + "lh",))
            tt("dve", ll, lgf[:, ti, :], lh, ALU.subtract, (lt, u + "lh"), (u + "ll",))
            if hgdbg == 19:
                continue
            mm(cs_[:, 0, :], trib[:], lh, True, False, (tribt, u + "lh"), ("ps3",))
            mm(cs_[:, 0, :], trib[:], ll, False, True, (tribt, u + "ll"), ("ps3",))
            mm(cs_[:, 1, :], blkb[:], lh, True, False, (blkbt, u + "lh"), ("ps3",))
            mm(cs_[:, 1, :], blkb[:], ll, False, True, (blkbt, u + "ll"), ("ps3",))
            if hgdbg == 20:
                continue
            cp("dve", b_sb, cs_[:, 0, :], ("ps3",), (u + "b",))
            act(enb, b_sb, AF.Exp, (u + "b",), (u + "enb",), scale=-1.0)
            if hgdbg == 21:
                continue
            tt("dve", dk, cs_[:, 1, :], b_sb, ALU.subtract, ("ps3", u + "b"), (u + "dk",))
            act(dk, dk, AF.Exp, (u + "dk",), (u + "dk",))
            tt("pool", kd_bf, kh[:, ti, :], dk, ALU.mult, (u + "kh%d" % ti, u + "dk"), (u + "kdbf",))
            if outputs:
                act(eb, b_sb, AF.Exp, (u + "b",), (u + "eb",))
                tt("dve", qt_bf, st_q[:, ti, :], eb, ALU.mult, (u + "stq%d" % ti, u + "eb"), (u + "qtbf",))
                tt("pool", kt_bf, kh[:, ti, :], enb, ALU.mult, (u + "kh%d" % ti, u + "enb"), (u + "ktbf",))
                pv = psum[2][:, 0:256].bitcast(BF16).rearrange("p (a b) -> p a b", a=4)
                for hh in range(2):
                    tr(pv[:, hh, :], qt_bf[:, hh * 128:(hh + 1) * 128], ident_b[:], (u + "qtbf", "ident_b"), ("ps2",))
                    tr(pv[:, 2 + hh, :], kt_bf[:, hh * 128:(hh + 1) * 128], ident_b[:], (u + "ktbf", "ident_b"),
                       ("ps2",))
                cp("act", qkT, pv, ("ps2",), (u + "qkT",))
            if hgdbg == 2:
                continue
            pe_ = psum[4][:, 0:64].rearrange("p (a b) -> p a b", a=2)
            cmo = 32 if sample else 0
            for hh in range(2):
                mm(pe_[:, hh, :], lh[:, hh * 128:(hh + 1) * 128], cmTb[:, cmo:cmo + 32], True, False,
                   (u + "lh", "cmTb"), ("ps4",))
                mm(pe_[:, hh, :], ll[:, hh * 128:(hh + 1) * 128], cmTb[:, cmo:cmo + 32], False, True,
                   (u + "ll", "cmTb"), ("ps4",))
            cp("dve", ebT[:, :, 0:ncm], pe_[:, :, 0:ncm], ("ps4",), (u + "ebT",))
            act(ebT[:, :, 0:ncm], ebT[:, :, 0:ncm], AF.Exp, (u + "ebT",), (u + "ebT",))
            if hgdbg == 3:
                continue
            for hh in range(2):
                hd = un * 2 + hh
                St = u + "S%d" % hh
                vsl = vh[:, ti, hh * 128:(hh + 1) * 128]
                vt = u + "vh%d" % ti
                o = psum[4][:, 256 + hh * 128:256 + (hh + 1) * 128]
                if outputs:
                    sc = psum[5][:, 0:128]
                    mm(sc, qkT[:, 2 + hh, :], qkT[:, hh, :], True, True, (u + "qkT",), ("ps5",))
                    tt("dve", scT, sc, tri[:], ALU.mult, ("ps5", trit), (u + "scT",))
                if not sample:
                    P.op("dve", lambda e, hh=hh: e.tensor_tensor(
                        kdm, kd_bf[:, hh * 128:(hh + 1) * 128].unsqueeze(1).to_broadcast([128, 8, 128]),
                        cmT[:, 0:8].unsqueeze(2).to_broadcast([128, 8, 128]), ALU.mult),
                        (u + "kdbf", "cmT"), (u + "kdm",))
                    for c in range(8):
                        mm(kvview(c), kdm[:, c, :], vsl, True, True, (u + "kdm", vt), ("pskv%d" % (c // 4),))
                    for c in range(8):
                        if hgdbg == 4:
                            break
                        stt("dve", Sb[hh][:, c + 1, :], Sb[hh][:, c, :], ebT[:, hh, c:c + 1], kvview(c),
                            ALU.mult, ALU.add, (St, "pskv%d" % (c // 4), u + "ebT"), (St,))
                    if outputs:
                        cp("pool", Sbb, Sb[hh][:, 0:8, :], (St,), (u + "Sbb",))
                        P.op("dve", lambda e, hh=hh: e.tensor_tensor(
                            qTm, qkT[:, hh, :].unsqueeze(1).to_broadcast([128, 8, 128]), cm16[:], ALU.mult),
                            (u + "qkT", "cm16"), (u + "qTm",))
                        mm(o, scT, vsl, True, False, (u + "scT", vt), ("ps4",))
                        for c in range(8):
                            mm(o, qTm[:, c, :], Sbb[:, c, :], False, c == 7, (u + "qTm", u + "Sbb"), ("ps4",))
                    cp("act", Sb[hh][:, 0, :], Sb[hh][:, 8, :], (St,), (St,))
                else:
                    s0t = u + "S0"
                    dma("st", S0, shg[:, hd].rearrange("s k v -> k s v"), (), (s0t,))
                    cp("pool", S0b, S0, (s0t,), (u + "S0b",))
                    o2 = psum[5][:, 256 + hh * 128:256 + (hh + 1) * 128]
                    mm(o, scT, vsl, True, False, (u + "scT", vt), ("ps4",))
                    for half in range(2):
                        P.op("dve", lambda e, hh=hh, half=half: e.tensor_tensor(
                            qTm, qkT[:, hh, :].unsqueeze(1).to_broadcast([128, 8, 128]),
                            cm4[:, half * 8:(half + 1) * 8, :], ALU.mult),
                            (u + "qkT", "cm4"), (u + "qTm",))
                        for s in range(8):
                            if half == 0:
                                mm(o, qTm[:, s, :], S0b[:, s, :], False, s == 7, (u + "qTm", u + "S0b"), ("ps4",))
                            else:
                                mm(o2, qTm[:, s, :], S0b[:, 8 + s, :], s == 0, s == 7, (u + "qTm", u + "S0b"),
                                   ("ps5",))
                        P.op("dve", lambda e, hh=hh, half=half: e.tensor_tensor(
                            kdm, kd_bf[:, hh * 128:(hh + 1) * 128].unsqueeze(1).to_broadcast([128, 8, 128]),
                            cmT[:, 8 + half * 8:16 + half * 8].unsqueeze(2).to_broadcast([128, 8, 128]), ALU.mult),
                            (u + "kdbf", "cmT"), (u + "kdm",))
                        for s in range(8):
                            mm(kvview(s), kdm[:, s, :], vsl, True, True, (u + "kdm", vt), ("pskv%d" % (s // 4),))
                        for s in range(8):
                            sq = half * 8 + s
                            stt("dve", Sn[:, sq, :], S0[:, sq, :], ebT[:, hh, sq:sq + 1], kvview(s),
                                ALU.mult, ALU.add, (s0t, "pskv%d" % (s // 4), u + "ebT"), (u + "Sn",))
                    dma("so", hgs_o[:, hd].rearrange("s k v -> k s v"), Sn, (u + "Sn",), ())
                if outputs:
                    ss = small[:, 10 + hh:11 + hh]
                    sst = u + "ss%d" % hh
                    osl = og_bf[:, hh * 128:(hh + 1) * 128]
                    cp("act", osum, o, ("ps4",), (u + "osum",))
                    if sample:
                        tt("dve", osum, o2, osum, ALU.add, ("ps5", u + "osum"), (u + "osum",))
                    osrc, osrct = osum, u + "osum"
                    act(osl, osrc, AF.Square, (osrct,), (u + "ogbf", sst), accum=ss)
                    rstd_from_ss(ss, 128, sst)
                    stt("dve", osl, osrc, ss, sg[:, ti, hh * 128:(hh + 1) * 128], ALU.mult, ALU.mult,
                        (osrct, sst, u + "sg%d" % ti), (u + "ogbf",))
            if outputs:
                pv2 = psum[2][:, 0:128].bitcast(BF16).rearrange("p (a b) -> p a b", a=2)
                for c in range(2):
                    tr(pv2[:, c, :], og_bf[:, c * 128:(c + 1) * 128], ident_b[:], (u + "ogbf", "ident_b"), ("ps2",))
                cp("act", ogTt, pv2, ("ps2",), (u + "ogT",))
                t0 = tok_off + ti * 128
                ch = 16 + un * 2
                dma("og", og_scr[ch:ch + 2, :, t0:t0 + 128].rearrange("c p t -> p c t"), ogTt,
                    (u + "ogT",), ("og_scr",))
        for hh in range(2):
            hd = un * 2 + hh
            St = u + "S%d" % hh
            dma("st", sh_scr[hd], Sb[hh][:, 0, :], (St,), ("sh_scr%d" % hd,))
            if last_store_out:
                dma("so", hgp_o[hd], Sb[hh][:, 0, :], (St,), ())

    def merge_phase(tok_off):
        W = R4
        s1 = a32(W, [5, 256]); W += 1280
        mg = a16(W, [5, 256]); W += 640
        assert W <= AW
        dma("og", ogT, og_scr[:, :, tok_off:tok_off + 640].rearrange("c p t -> p c t"), ("og_scr",), ("ogT",))
        for j in range(16):
            def ev_gr(ti, ps, pt):
                act(s1[:, ti, :], ps, AF.Sigmoid, (pt,), ("s1_%d" % ti,))

            def ev_pr(ti, ps, pt):
                tt("dve", s1[:, ti, :], ps, s1[:, ti, :], ALU.mult, (pt, "s1_%d" % ti), ("s1_%d" % ti,))

            m2 = a32(R4 + 1920, [5, 256])

            def ev_gh(ti, ps, pt):
                act(m2[:, ti, :], ps, AF.Sigmoid, (pt,), ("m2_%d" % ti,))

            def ev_ph(ti, ps, pt, j=j):
                tt("dve", m2[:, ti, :], ps, m2[:, ti, :], ALU.mult, (pt, "m2_%d" % ti), ("m2_%d" % ti,))
                tt("pool", mg[:, ti, :], m2[:, ti, :], s1[:, ti, :], ALU.add, ("m2_%d" % ti, "s1_%d" % ti),
                   ("mg_%d" % ti,))
                pv = psum[2][:, 0:128].bitcast(BF16).rearrange("p (a b) -> p a b", a=2)
                for c in range(2):
                    tr(pv[:, c, :], mg[:, ti, c * 128:(c + 1) * 128], ident_b[:], ("mg_%d" % ti, "ident_b"), ("ps2",))
                cp("act", mgT[:, j * 2:j * 2 + 2, ti * 128:(ti + 1) * 128], pv, ("ps2",), ("mgT",))

            proj(xnT, "actT", 5, w_in[:, C_GATE_R + j * 256:C_GATE_R + (j + 1) * 256], 256, ev_gr)
            proj(ogT, "ogT", 5, w_br_ret[:, j * 256:(j + 1) * 256], 256, ev_pr, kcn=16, koff=0)
            proj(xnT, "actT", 5, w_in[:, C_GATE_H + j * 256:C_GATE_H + (j + 1) * 256], 256, ev_gh)
            proj(ogT, "ogT", 5, w_br_hg[:, j * 256:(j + 1) * 256], 256, ev_ph, kcn=16, koff=16)

    def wout_phase(tiles):
        for li, tg in enumerate(tiles):
            dma("x", x1[:, li, :], xa[tg * 128:(tg + 1) * 128, :], (), ("x1_%d" % li,))
        for j in range(16):
            def ev(ti, ps, pt, j=j):
                tt("dve", x1[:, ti, j * 256:(j + 1) * 256], ps, x1[:, ti, j * 256:(j + 1) * 256], ALU.add,
                   (pt, "x1_%d" % ti), ("x1_%d" % ti,))
            proj(mgT, "mgT", 5, w_out[:, j * 256:(j + 1) * 256], 256, ev)

    def ffn_phase(grp):
        W = R4
        ue = a32(W, [644]); W += 644
        cc = a32(W, [640]); W += 640
        hT = a16(W, [2, 640]); W += 640
        ues = a32(W, [16, 6]); W += 96
        ccs = a32(W, [16, 4]); W += 64
        ctp = a32(W, [34]); W += 34
        W += (-W) % 2
        scv = a32(W, [256]); W += 256
        ct = a32(W, [256]); W += 256
        scv3 = [a16(W + i * 128, [256]) for i in range(3)]; W += 384
        scvr = a32(W, [256]); W += 256
        ctp3 = [a16(W + i * 17, [34]) for i in range(3)]; W += 52
        ctpr = a32(W, [34]); W += 34

        def split3(src, parts, res, srct, dstt):
            cp("pool", parts[0], src, (srct,), (dstt,))
            tt("dve", res, src, parts[0], ALU.subtract, (srct, dstt), (dstt + "r",))
            cp("pool", parts[1], res, (dstt + "r",), (dstt,))
            tt("dve", res, res, parts[1], ALU.subtract, (dstt + "r", dstt), (dstt + "r",))
            cp("pool", parts[2], res, (dstt + "r",), (dstt,))
        assert W <= AW, W
        down_tiles = [1, 2, 3, 4] if grp == 0 else [0, 1, 2, 3, 4]
        for fb in range(43):
            f0 = fb * 256
            wg, wgt = load_w(w_gate[:, f0:f0 + 256], 32, 256)
            wu, wut = load_w(w_up[:, f0:f0 + 256], 32, 256)
            wd, wdt = load_w(w_down[f0:f0 + 256, :], 2, 4096)
            if grp == 1:
                dma("cv", scv[0:32, :], sconv[:, f0:f0 + 256], (), ("scv",))
                split3(scv[0:32, :], [t_[0:32, :] for t_ in scv3], scvr[0:32, :], "scv", "scv3")
            for sub in range(2):
                fc = fb * 2 + sub
                for hf in range(2):
                    ps = psum[hf][:, 0:320]
                    for kc in range(32):
                        mm(ps, wg[:, kc, sub * 128:(sub + 1) * 128], xn2T[:, kc, hf * 320:(hf + 1) * 320],
                           kc == 0, kc == 31, (wgt, "actT2"), ("ps%d" % hf,))
                    cp("act", ue[:, 2 + hf * 320:2 + (hf + 1) * 320], ps, ("ps%d" % hf,), ("ue",))
                cp("pool", ue[:, 0:2], carry[:, fc, :], ("carry",), ("ue",))
                w0, w1_, w2, bb = (cwb[:, fc, 0:1], cwb[:, fc, 1:2], cwb[:, fc, 2:3], cwb[:, fc, 3:4])
                act(cc, ue[:, 2:642], AF.Identity, ("ue", "cw"), ("cc",), scale=w2, bias=bb)
                stt("dve", cc, ue[:, 1:641], w1_, cc, ALU.mult, ALU.add, ("ue", "cw", "cc"), ("cc",))
                stt("dve", cc, ue[:, 0:640], w0, cc, ALU.mult, ALU.add, ("ue", "cw", "cc"), ("cc",))
                if grp == 0:
                    cp("pool", carry[:, fc, :], ue[:, 640:642], ("ue",), ("carry",))
                else:
                    pst = psum[3][:, 256:288]
                    for q3 in range(3):
                        mm(pst, scv3[q3][0:32, sub * 128:(sub + 1) * 128], ident_b[0:32, 0:32], q3 == 0, q3 == 2,
                           ("scv3", "ident_b"), ("ps3",))
                    cp("act", ues[:, :, 0:2], pst.rearrange("p (s j) -> p s j", j=2), ("ps3",), ("ues",))
                    cp("pool", ues[:, :, 2:6], ue[:, 514:578].rearrange("p (s j) -> p s j", j=4), ("ue",), ("ues",))
                    act(ccs, ues[:, :, 2:6], AF.Identity, ("ues", "cw"), ("ccs",), scale=w2, bias=bb)
                    stt("dve", ccs, ues[:, :, 1:5], w1_, ccs, ALU.mult, ALU.add, ("ues", "cw", "ccs"), ("ccs",))
                    stt("dve", ccs, ues[:, :, 0:4], w0, ccs, ALU.mult, ALU.add, ("ues", "cw", "ccs"), ("ccs",))
                    cp("pool", cc[:, 512:576].rearrange("p (s j) -> p s j", j=4), ccs, ("ccs", "cc"), ("cc",))
                    cp("pool", ctp[:, 0:32].rearrange("p (s j) -> p s j", j=2), ues[:, :, 4:6], ("ues",), ("ctp",))
                    cp("pool", ctp[:, 32:34], ue[:, 512:514], ("ue",), ("ctp",))
                    split3(ctp, ctp3, ctpr, "ctp", "ctp3")
                act(cc, cc, AF.Silu, ("cc",), ("cc",))
                for hf in range(2):
                    ps = psum[hf][:, 0:320]
                    for kc in range(32):
                        mm(ps, wu[:, kc, sub * 128:(sub + 1) * 128], xn2T[:, kc, hf * 320:(hf + 1) * 320],
                           kc == 0, kc == 31, (wut, "actT2"), ("ps%d" % hf,))
                    tt("dve", hT[:, sub, hf * 320:(hf + 1) * 320], ps, cc[:, hf * 320:(hf + 1) * 320], ALU.mult,
                       ("ps%d" % hf, "cc"), ("hT",))
                if grp == 1:
                    pso = psum[3][0:34, 0:128]
                    for q3 in range(3):
                        mm(pso, ctp3[q3], ident_b[:], q3 == 0, q3 == 2, ("ctp3", "ident_b"), ("ps3",))
                    cp("act", ct[0:34, sub * 128:(sub + 1) * 128], pso, ("ps3",), ("ct",))
            if grp == 1:
                dma("cv", convs_o[:, f0:f0 + 256], ct[0:32, :], ("ct",), ())
                dma("cv", convp_o[:, f0:f0 + 256], ct[32:34, :], ("ct",), ())
            n = 0
            for ti in down_tiles:
                for cb in range(8):
                    pb = 4 + (n % 2)
                    n += 1
                    ps = psum[pb][:, 0:512]
                    for sub in range(2):
                        mm(ps, hT[:, sub, ti * 128:(ti + 1) * 128], wd[:, sub, cb * 512:(cb + 1) * 512],
                           sub == 0, sub == 1, ("hT", wdt), ("ps%d" % pb,))
                    tt("dve", x1[:, ti, cb * 512:(cb + 1) * 512], ps, x1[:, ti, cb * 512:(cb + 1) * 512], ALU.add,
                       ("ps%d" % pb, "x1_%d" % ti), ("x1_%d" % ti,))
        P.barrier()
        gfin = a32(R2, [4096])
        junk = a16(R2 + 4096, [4096])
        dma("c", gfin, g_fin.to_broadcast([128, D]), (), ("gfin",))
        for ti in down_tiles:
            ss = small[:, 20 + ti:21 + ti]
            sst = "fss%d" % ti
            act(junk, x1[:, ti, :], AF.Square, ("x1_%d" % ti,), ("junk", sst), accum=ss)
            rstd_from_ss(ss, D, sst)
            stt("dve", x1[:, ti, :], x1[:, ti, :], ss, gfin, ALU.mult, ALU.mult, ("x1_%d" % ti, sst, "gfin"),
                ("x1_%d" % ti,))
            if grp == 0:
                r0, nr = (ti - 1) * 128, 128
            elif ti < 4:
                r0, nr = 512 + ti * 128, 128
            else:
                r0, nr = 1024, 64
            dma("y", y_o[r0:r0 + nr, :], x1[0:nr, ti, :], ("x1_%d" % ti,), ())

    import os
    kstop = int(os.environ.get("KSTOP", "999"))
    phase_no = [0]

    def phase_ok():
        phase_no[0] += 1
        return phase_no[0] <= kstop

    load_consts()
    pre_tiles = list(range(7))
    xnT_p = a16(R0, [32, 896])

    def with_base(fn, base, *a, **k):
        nonlocal R1
        old = R1
        R1 = base
        try:
            fn(*a, **k)
        finally:
            R1 = old

    def prog():
        nonlocal xnT
        if not phase_ok():
            return
        norm_phase(pre_tiles, xnT_p, gmixT, "gmixT", wbase=R2)
        P.barrier()
        xnT_save = xnT
        xnT = xnT_p
        if phase_ok():
            for h in range(8):
                with_base(ret_unit, 60 * KW, h, pre_tiles, False, True, False, 0)
            P.barrier()
        if phase_ok():
            for un in range(8):
                with_base(hg_unit, 60 * KW, un, pre_tiles, False, True, False, 0)
            P.barrier()
        xnT = xnT_save
        for grp in range(2):
            tiles = list(range(7, 12)) if grp == 0 else list(range(12, 17))
            tok_off = grp * 640
            if not phase_ok():
                return
            norm_phase(tiles, xnT, gmixT, "gmixT")
            P.barrier()
            if not phase_ok():
                return
            for h in range(8):
                ret_unit(h, tiles, True, False, grp == 1, tok_off)
            P.barrier()
            if not phase_ok():
                return
            for un in range(8):
                hg_unit(un, tiles, True, False, grp == 1, tok_off)
            P.barrier()
            if not phase_ok():
                return
            merge_phase(tok_off)
            P.barrier()
            if not phase_ok():
                return
            wout_phase(tiles)
            P.barrier()
            if not phase_ok():
                return
            norm_phase([0, 1, 2, 3, 4], xn2T, gffnT, "gffnT", src_x1=True, wbase=R4 - 8192, dtok="actT2")
            P.barrier()
            if not phase_ok():
                return
            ffn_phase(grp)
            P.barrier()

    prog()
    P.barrier()

    with nc.Block() as block:
        P.emit(nc, block, esem, dsem)
    es.close()
    return nc


_NC_CACHE = {}


def _prep(inputs):
    f = lambda k: np.ascontiguousarray(np.asarray(inputs[k], dtype=np.float32))
    xp = f("x_prompt")
    xs = f("x_sample")
    consts = _consts()
    shared = {
        "w_in": f("w_in")[0], "w_br_ret": f("w_br_ret")[0], "w_br_hg": f("w_br_hg")[0], "w_out": f("w_out")[0],
        "w_gate": f("w_gate")[0], "w_up": f("w_up")[0], "w_down": f("w_down")[0],
        "g_mix": np.ascontiguousarray(f("norm_mix_g")[0].reshape(32, 128).T),
        "g_ffn": np.ascontiguousarray(f("norm_ffn_g")[0].reshape(32, 128).T),
        "g_fin": f("final_norm_g").reshape(1, D),
        "ret_g": f("ret_norm_g")[0].reshape(1, 2048), "hg_g": f("hg_norm_g")[0].reshape(1, 2048),
        "lbl": f("hg_lb_logits"),
        "k_dm": consts["dm"], "k_dm4": consts["dm4"], "k_dec": consts["dec"],
        "k_tri16": consts["tri16"], "k_blk16": consts["blk16"], "k_tri4": consts["tri4"], "k_blk4": consts["blk4"],
        "k_cm16": consts["cm16"], "k_cm4": consts["cm4"], "k_cmT": consts["cmT"], "k_cmT64": consts["cmT64"], "k_ident": consts["ident"],
    }
    cw = f("conv_w")[0]
    cb = f("conv_b")[0]
    cwb = np.concatenate([cw, cb[None, :]], axis=0)
    shared["cwb"] = np.ascontiguousarray(cwb.reshape(4, 86, 128).transpose(2, 1, 0).reshape(128, 86 * 4))
    sret = f("state_ret")[0]
    shg = f("state_hgrn")[0]
    sconv = f("state_ffn_conv")[0]
    in_maps = []
    for c in range(8):
        b, half = c // 2, c % 2
        xa = np.zeros((NT_ALL * 128, D), np.float32)
        pos = np.zeros((NT_ALL * 128,), np.float32)
        if half == 1:
            xa[0:1024] = xp[b, 0:1024]
            pos[0:1024] = np.arange(1024)
            xa[1024:2048] = xp[b, 1024:2048]
            pos[1024:2048] = np.arange(1024, 2048)
        else:
            xa[1024:2048] = xp[b, 0:1024]
            pos[1024:2048] = np.arange(1024)
        xa[2048:2112] = xs[16 * c:16 * c + 16].reshape(64, D)
        pos[2048:2112] = PAST + (np.arange(64) % 4)
        cs, sn = _rope_tables(pos)
        m = dict(shared)
        m["xa"] = xa
        m["cosT"] = np.ascontiguousarray(cs.reshape(NT_ALL, 128, 128))
        m["sinT"] = np.ascontiguousarray(sn.reshape(NT_ALL, 128, 128))
        m["sret"] = np.ascontiguousarray(sret[16 * c:16 * c + 16])
        m["shg"] = np.ascontiguousarray(shg[16 * c:16 * c + 16])
        m["sconv"] = np.ascontiguousarray(sconv[16 * c:16 * c + 16].reshape(32, FF))
        in_maps.append(m)
    return in_maps


def kernel(**inputs):
    in_maps = _prep(inputs)
    if "nc" not in _NC_CACHE:
        _NC_CACHE["nc"] = build_program()
    nc = _NC_CACHE["nc"]
    res = run_bass_kernel_spmd(nc, in_maps, core_ids=list(range(8)))
    return _assemble(res.results)


def _assemble(R):
    y_prompt = np.zeros((4, 2048, D), np.float32)
    y_sample = np.zeros((128, 4, D), np.float32)
    ret_p = np.zeros((1, 4, 8, 256, 256), np.float32)
    hg_p = np.zeros((1, 4, 16, 128, 128), np.float32)
    conv_p = np.zeros((1, 4, 2, FF), np.float32)
    ret_s = np.zeros((1, 128, 8, 256, 256), np.float32)
    hg_s = np.zeros((1, 128, 16, 128, 128), np.float32)
    conv_s = np.zeros((1, 128, 2, FF), np.float32)
    for c in range(8):
        b, half = c // 2, c % 2
        r = R[c]
        y_prompt[b, half * 1024:(half + 1) * 1024] = r["y_o"][0:1024]
        y_sample[16 * c:16 * c + 16] = r["y_o"][1024:1088].reshape(16, 4, D)
        ret_s[0, 16 * c:16 * c + 16] = r["rets_o"]
        hg_s[0, 16 * c:16 * c + 16] = r["hgs_o"]
        conv_s[0, 16 * c:16 * c + 16] = r["convs_o"].reshape(16, 2, FF)
        if half == 1:
            ret_p[0, b] = r["retp_o"]
            hg_p[0, b] = r["hgp_o"]
            conv_p[0, b] = r["convp_o"]
    return (y_prompt, y_sample, ret_p, hg_p, conv_p, ret_s, hg_s, conv_s)
```
